# Optimizing a Trainium2 kernel written in Bass

```python
import jax
import jax.numpy as jnp
from jax import lax
import numpy as np

D_MODEL = 1024
BATCH = 8
SEQ = 2048
DEPTH = 1

GRID_W = 64
MEM_LEN = 256
N_BRANCHES = 3
RET_HEADS = 4
RET_QK_DIM = 128
RET_V_DIM = 256
RET_CHUNK = 128
ROPE_BASE = 10000.0
NA_HEADS = 8
NA_HEAD_DIM = 64
NA_ROWS = 8
NA_COLS = 16
XA_HEADS = 4
XA_HEAD_DIM = 128
MOE_GROUPS = 4
MOE_EXPERTS_PER_GROUP = 4
MOE_TOP_K = 2
MOE_D_FF = 512
RMS_EPS = 1e-6
GN_EPS = 1e-5

RET_QK_WIDTH = RET_HEADS * RET_QK_DIM
RET_V_WIDTH = RET_HEADS * RET_V_DIM
NA_WIDTH = NA_HEADS * NA_HEAD_DIM
XA_WIDTH = XA_HEADS * XA_HEAD_DIM
IN_SPLITS = (RET_QK_WIDTH, RET_QK_WIDTH, RET_V_WIDTH, RET_V_WIDTH, NA_WIDTH, NA_WIDTH, NA_WIDTH, XA_WIDTH, N_BRANCHES * D_MODEL)
IN_WIDTH = 2 * RET_QK_WIDTH + 2 * RET_V_WIDTH + 3 * NA_WIDTH + XA_WIDTH + N_BRANCHES * D_MODEL

kernel_name = 'hybrid_retention_natten_memory_hmoe_encoder'


def _rmsnorm(x, g):
    xf = x.astype(jnp.float32)
    y = xf * lax.rsqrt(jnp.mean(xf * xf, axis=-1, keepdims=True) + RMS_EPS)
    return (y * g.astype(jnp.float32)).astype(x.dtype)


def _rope(a):
    seq, dh = a.shape[1], a.shape[-1]
    half = dh // 2
    inv_freq = ROPE_BASE ** (-jnp.arange(half, dtype=jnp.float32) / half)
    ang = jnp.arange(seq, dtype=jnp.float32)[:, None] * inv_freq[None, :]
    cos = jnp.cos(ang)[None, :, None, :]
    sin = jnp.sin(ang)[None, :, None, :]
    a1, a2 = a[..., :half], a[..., half:]
    return jnp.concatenate([a1 * cos - a2 * sin, a1 * sin + a2 * cos], axis=-1)


def _retention_one_direction(q, k, v, log_gamma, strict):
    b, h, s, dk = q.shape
    dv = v.shape[-1]
    c = RET_CHUNK
    n = s // c
    qc = q.reshape(b, h, n, c, dk)
    kc = k.reshape(b, h, n, c, dk)
    vc = v.reshape(b, h, n, c, dv)
    pos = jnp.arange(c, dtype=jnp.float32)
    diff = pos[:, None] - pos[None, :]
    mask = (diff > 0) if strict else (diff >= 0)
    decay = jnp.where(mask[None], jnp.exp(jnp.where(mask, diff, 0.0)[None] * log_gamma[:, None, None]), 0.0)
    scores = jnp.einsum('bhnid,bhnjd->bhnij', qc, kc) * decay[None, :, None]
    y_inner = jnp.einsum('bhnij,bhnje->bhnie', scores, vc)
    k_decay = jnp.exp((c - 1 - pos)[None, :] * log_gamma[:, None])
    q_decay = jnp.exp((pos + 1)[None, :] * log_gamma[:, None])
    chunk_decay = jnp.exp(c * log_gamma)[None, :, None, None]
    kv = jnp.einsum('bhnjd,hj,bhnje->bhnde', kc, k_decay, vc)

    def step(state, kv_chunk):
        return state * chunk_decay + kv_chunk, state

    _, prev = lax.scan(step, jnp.zeros((b, h, dk, dv), jnp.float32), jnp.moveaxis(kv, 2, 0))
    prev = jnp.moveaxis(prev, 0, 2)
    y_cross = jnp.einsum('bhnid,hi,bhnde->bhnie', qc, q_decay, prev)
    return (y_inner + y_cross).reshape(b, h, s, dv)


def _retention_branch(rq, rk, rv, rg, dec_f, dec_b, gn_gain, w_o):
    b, s, _ = rq.shape
    q = _rope(rq.reshape(b, s, RET_HEADS, RET_QK_DIM).astype(jnp.float32)) * (RET_QK_DIM ** -0.5)
    k = _rope(rk.reshape(b, s, RET_HEADS, RET_QK_DIM).astype(jnp.float32))
    v = rv.reshape(b, s, RET_HEADS, RET_V_DIM).astype(jnp.float32)
    q, k, v = q.transpose(0, 2, 1, 3), k.transpose(0, 2, 1, 3), v.transpose(0, 2, 1, 3)
    lg_f = jax.nn.log_sigmoid(dec_f.astype(jnp.float32))
    lg_b = jax.nn.log_sigmoid(dec_b.astype(jnp.float32))
    y_fwd = _retention_one_direction(q, k, v, lg_f, False)
    y_bwd = jnp.flip(_retention_one_direction(jnp.flip(q, 2), jnp.flip(k, 2), jnp.flip(v, 2), lg_b, True), 2)
    y = y_fwd + y_bwd
    mu = jnp.mean(y, axis=-1, keepdims=True)
    var = jnp.mean(jnp.square(y - mu), axis=-1, keepdims=True)
    y = (y - mu) * lax.rsqrt(var + GN_EPS)
    y = y.transpose(0, 2, 1, 3).reshape(b, s, RET_V_WIDTH) * gn_gain.astype(jnp.float32)
    return (jax.nn.silu(rg) * y.astype(rg.dtype)) @ w_o


def _neighbourhood_attention(nq, nk, nv, rpb):
    b, s, _ = nq.shape
    rows_n = s // GRID_W
    w = GRID_W
    kr = min(NA_ROWS, rows_n)
    kc = NA_COLS

    def to_grid(a):
        return a.reshape(b, rows_n, w, NA_HEADS, NA_HEAD_DIM).transpose(0, 3, 1, 2, 4)

    qg, kg, vg = to_grid(nq), to_grid(nk), to_grid(nv)
    rows = jnp.arange(rows_n)
    row_start = jnp.clip(rows - kr // 2, 0, rows_n - kr)
    row_idx = row_start[:, None] + jnp.arange(kr)[None, :]
    cols = jnp.arange(w)
    col_start = jnp.clip(cols - kc // 2, 0, w - kc)
    col_off = cols[None, :] - col_start[:, None]
    col_mask = (col_off >= 0) & (col_off < kc)
    k_rows = jnp.take(kg, row_idx, axis=2)
    v_rows = jnp.take(vg, row_idx, axis=2)
    sc = jnp.einsum('bhrqd,bhrwkd->bhrqwk', qg, k_rows).astype(jnp.float32) * (NA_HEAD_DIM ** -0.5)
    rel_r = row_idx - rows[:, None] + (NA_ROWS - 1)
    rel_c = jnp.clip(cols[None, :] - cols[:, None], -(kc - 1), kc - 1) + (kc - 1)
    bias = rpb[:, rel_r[:, None, :, None], rel_c[None, :, None, :]].astype(jnp.float32)
    sc = jnp.where(col_mask[:, None, :], sc + bias, -jnp.inf)
    p = jax.nn.softmax(sc, axis=(-2, -1)).astype(vg.dtype)
    o = jnp.einsum('bhrqwk,bhrwkd->bhrqd', p, v_rows)
    return o.transpose(0, 2, 3, 1, 4).reshape(b, s, NA_WIDTH)


def _memory_cross_attention(xq, mem_n, w_kv):
    b, s, _ = xq.shape
    m = mem_n.shape[1]
    mk, mv = jnp.split(mem_n @ w_kv, 2, axis=-1)
    q = xq.reshape(b, s, XA_HEADS, XA_HEAD_DIM)
    k = mk.reshape(b, m, XA_HEADS, XA_HEAD_DIM)
    v = mv.reshape(b, m, XA_HEADS, XA_HEAD_DIM)
    sc = jnp.einsum('bshd,bmhd->bhsm', q, k).astype(jnp.float32) * (XA_HEAD_DIM ** -0.5)
    p = jax.nn.softmax(sc, axis=-1).astype(v.dtype)
    return jnp.einsum('bhsm,bmhd->bshd', p, v).reshape(b, s, XA_WIDTH)


def _hierarchical_moe(h, w_rg, b_rg, w_re, b_re, w_gate, w_up, w_down):
    b, s, d = h.shape
    t = h.reshape(b * s, d)
    g_n, e_n = MOE_GROUPS, MOE_EXPERTS_PER_GROUP
    grp_p = jax.nn.softmax((t @ w_rg + b_rg).astype(jnp.float32), axis=-1)
    grp_w, grp_idx = lax.top_k(grp_p, 1)
    exp_logits = (t @ w_re + b_re).astype(jnp.float32).reshape(-1, g_n, e_n)
    in_grp = jnp.take_along_axis(exp_logits, grp_idx[:, :, None], axis=1)[:, 0]
    top_w, top_idx = lax.top_k(jax.nn.softmax(in_grp, axis=-1), MOE_TOP_K)
    top_w = top_w / jnp.sum(top_w, axis=-1, keepdims=True) * grp_w
    exp_w = jnp.sum(jax.nn.one_hot(top_idx, e_n, dtype=jnp.float32) * top_w[..., None], axis=1)
    combine = (jax.nn.one_hot(grp_idx[:, 0], g_n, dtype=jnp.float32)[:, :, None] * exp_w[:, None, :]).astype(t.dtype)
    out = jnp.zeros_like(t)
    for g in range(g_n):
        a = jnp.einsum('td,edf->tef', t, w_gate[g])
        u = jnp.einsum('td,edf->tef', t, w_up[g])
        hid = jax.nn.silu(a) * u * combine[:, g, :, None]
        out = out + jnp.einsum('tef,efd->td', hid, w_down[g])
    return out.reshape(b, s, d)


def setup_inputs(seed: int = 0) -> dict:
    key = jax.random.key(seed)
    ks = jax.random.split(key, 24)
    f32 = jnp.float32
    d = D_MODEL
    n_l = DEPTH
    g_n, e_n, f_n = MOE_GROUPS, MOE_EXPERTS_PER_GROUP, MOE_D_FF

    def nrm(k, shape, fan_in):
        return jax.random.normal(k, shape, f32) * (fan_in ** -0.5)

    def gain(k, shape):
        return 1.0 + 0.02 * jax.random.normal(k, shape, f32)

    base_gamma = 1.0 - 2.0 ** (-5.0 - jnp.arange(RET_HEADS, dtype=f32))
    base_logit = jnp.log(base_gamma) - jnp.log1p(-base_gamma)
    return {
        'x': jax.random.normal(ks[0], (BATCH, SEQ, d), f32),
        'mem': jax.random.normal(ks[1], (BATCH, MEM_LEN, d), f32),
        'g_mix': gain(ks[2], (n_l, d)),
        'w_in': nrm(ks[3], (n_l, d, IN_WIDTH), d),
        'ret_decay_fwd': base_logit[None] + 0.05 * jax.random.normal(ks[4], (n_l, RET_HEADS), f32),
        'ret_decay_bwd': base_logit[None] + 0.05 * jax.random.normal(ks[5], (n_l, RET_HEADS), f32),
        'ret_norm_gain': gain(ks[6], (n_l, RET_V_WIDTH)),
        'w_ret_o': nrm(ks[7], (n_l, RET_V_WIDTH, d), RET_V_WIDTH),
        'na_rpb': 0.1 * jax.random.normal(ks[8], (n_l, NA_HEADS, 2 * NA_ROWS - 1, 2 * NA_COLS - 1), f32),
        'w_na_o': nrm(ks[9], (n_l, NA_WIDTH, d), NA_WIDTH),
        'g_mem': gain(ks[10], (n_l, d)),
        'w_mem_kv': nrm(ks[11], (n_l, d, 2 * XA_WIDTH), d),
        'w_xa_o': nrm(ks[12], (n_l, XA_WIDTH, d), XA_WIDTH),
        'w_out': nrm(ks[13], (n_l, d, d), d),
        'g_ffn': gain(ks[14], (n_l, d)),
        'w_router_group': nrm(ks[15], (n_l, d, g_n), d),
        'b_router_group': 0.01 * jax.random.normal(ks[16], (n_l, g_n), f32),
        'w_router_expert': nrm(ks[17], (n_l, d, g_n * e_n), d),
        'b_router_expert': 0.01 * jax.random.normal(ks[18], (n_l, g_n * e_n), f32),
        'w_exp_gate': nrm(ks[19], (n_l, g_n, e_n, d, f_n), d),
        'w_exp_up': nrm(ks[20], (n_l, g_n, e_n, d, f_n), d),
        'w_exp_down': nrm(ks[21], (n_l, g_n, e_n, f_n, d), f_n),
        'g_final': gain(ks[22], (d,)),
    }


def reference(x, mem, g_mix, w_in, ret_decay_fwd, ret_decay_bwd, ret_norm_gain, w_ret_o, na_rpb, w_na_o, g_mem, w_mem_kv, w_xa_o, w_out, g_ffn, w_router_group, b_router_group, w_router_expert, b_router_expert, w_exp_gate, w_exp_up, w_exp_down, g_final):
    split_points = [int(p) for p in np.cumsum(np.array(IN_SPLITS))[:-1]]
    for l in range(DEPTH):
        h = _rmsnorm(x, g_mix[l])
        rq, rk, rv, rg, nq, nk, nv, xq, gate_logits = jnp.split(h @ w_in[l], split_points, axis=-1)
        y_ret = _retention_branch(rq, rk, rv, rg, ret_decay_fwd[l], ret_decay_bwd[l], ret_norm_gain[l], w_ret_o[l])
        y_na = _neighbourhood_attention(nq, nk, nv, na_rpb[l]) @ w_na_o[l]
        y_xa = _memory_cross_attention(xq, _rmsnorm(mem, g_mem[l]), w_mem_kv[l]) @ w_xa_o[l]
        g_ret, g_na, g_xa = jnp.split(jax.nn.sigmoid(gate_logits), N_BRANCHES, axis=-1)
        x = x + (g_ret * y_ret + g_na * y_na + g_xa * y_xa) @ w_out[l]
        x = x + _hierarchical_moe(_rmsnorm(x, g_ffn[l]), w_router_group[l], b_router_group[l], w_router_expert[l], b_router_expert[l], w_exp_gate[l], w_exp_up[l], w_exp_down[l])
    return _rmsnorm(x, g_final)
```

```python
import numpy as np
from contextlib import ExitStack
import concourse.bass as bass
import concourse.mybir as mybir
from concourse.bass_utils import run_bass_kernel_spmd

F32 = mybir.dt.float32
BF16 = mybir.dt.bfloat16
AF = mybir.ActivationFunctionType
ALU = mybir.AluOpType
AX = mybir.AxisListType

D = 1024
S = 2048
NT = 16
KC = 8
MEM = 256
NEGBIG = -240000.0
QSCALE = 128.0 ** -0.5


class Tok:
    __slots__ = ("sem", "val")

    def __init__(self, sem=None, val=None):
        self.sem = sem
        self.val = val


class Buf:
    __slots__ = ("name", "w", "r")

    def __init__(self, name=""):
        self.name = name
        self.w = None
        self.r = []

    def add_read(self, tok):
        self.r.append(tok)
        if len(self.r) > 24:
            best = {}
            keep = []
            for t in self.r:
                if t.val is None:
                    keep.append(t)
                else:
                    k = id(t.sem)
                    if k not in best or best[k].val < t.val:
                        best[k] = t
            self.r = keep + list(best.values())


class Eng:
    def __init__(self, name, h, sem):
        self.name = name
        self.h = h
        self.sem = sem
        self.count = 0
        self.pending = []
        self.waited = {}
        self.nins = 0

    def wait(self, tok):
        if tok is None:
            return
        assert tok.val is not None, f"unresolved token waited by {self.name}"
        key = id(tok.sem)
        if self.waited.get(key, 0) >= tok.val:
            return
        self.h.wait_ge(tok.sem, tok.val)
        self.waited[key] = tok.val

    def _deps(self, reads, writes):
        deps = []
        for b in reads:
            if b.w is not None:
                deps.append(b.w)
        for b in writes:
            deps.extend(b.r)
            if b.w is not None:
                deps.append(b.w)
        return deps

    def op(self, fn, reads=(), writes=(), sig=True):
        for d in self._deps(reads, writes):
            if d.val is None:
                assert any(d is p for p in self.pending), f"unresolved dep on {self.name}"
                continue
            self.wait(d)
        ins = fn()
        self.nins += 1
        tok = Tok(self.sem, None)
        self.pending.append(tok)
        if sig:
            self.count += 1
            ins.then_inc(self.sem, 1)
            for t in self.pending:
                t.val = self.count
            self.pending = []
        for b in reads:
            b.add_read(tok)
        for b in writes:
            b.w = tok
            b.r = []
        return tok


class DmaQ:
    def __init__(self, eng, sems):
        self.eng = eng
        self.sems = sems
        self.cnt = [0] * len(sems)
        self.i = 0

    def dma(self, out, in_, reads=(), writes=(), **kw):
        e = self.eng
        for d in e._deps(reads, writes):
            e.wait(d)
        k = self.i % len(self.sems)
        self.i += 1
        sem = self.sems[k]
        if self.cnt[k] > 0:
            e.wait(Tok(sem, self.cnt[k]))
        self.cnt[k] += 16
        e.h.dma_start(out=out, in_=in_, **kw).then_inc(sem, 16)
        tok = Tok(sem, self.cnt[k])
        for b in reads:
            b.add_read(tok)
        for b in writes:
            b.w = tok
            b.r = []
        return tok


def _dt_size(dt):
    return 2 if dt == BF16 else 4


class Arena:
    def __init__(self, tensor, lo, hi):
        self.t = tensor
        self.lo = lo
        self.hi = hi
        self.top = lo

    def alloc(self, shape, dt):
        n = 1
        for s in shape:
            n *= s
        nb = n * _dt_size(dt)
        nb = (nb + 63) // 64 * 64
        off = self.top
        assert off + nb <= self.hi, f"arena overflow: need {nb} at {off - self.lo} of {self.hi - self.lo}"
        self.top = off + nb
        ap = self.t[:, off // 4:(off + nb) // 4]
        if dt != F32:
            ap = ap.bitcast(dt)
        ap = ap[:, 0:n]
        if len(shape) == 1:
            return ap
        names = [f"d{i}" for i in range(len(shape))]
        pat = "p (" + " ".join(names) + ") -> p " + " ".join(names)
        kw = {names[i]: shape[i] for i in range(1, len(shape))}
        return ap.rearrange(pat, **kw)

    def mark(self):
        return self.top

    def release(self, m):
        self.top = m


def _host_constants():
    c = {}
    c["c_ident"] = np.eye(128, dtype=np.float32)
    half = 64
    inv_freq = (10000.0 ** (-np.arange(half, dtype=np.float32) / half)).astype(np.float32)
    pos = np.arange(S, dtype=np.float32)
    ang = (pos[:, None] * inv_freq[None, :]).astype(np.float32)
    rope = np.stack([np.cos(ang), np.sin(ang)], axis=1).astype(np.float32)
    c["c_rope"] = np.ascontiguousarray(rope.reshape(NT, 128, 2, 64).transpose(1, 0, 2, 3))
    j = np.arange(128, dtype=np.float32)[:, None]
    i = np.arange(128, dtype=np.float32)[None, :]
    ret = np.zeros((128, 6, 128), np.float32)
    ret[:, 0, :] = -np.maximum(i - j, 0)
    ret[:, 1, :] = (i >= j)
    ret[:, 2, :] = -np.maximum(j - i, 0)
    ret[:, 3, :] = (i < j)
    ret[:, 4, :] = -(i + 1)
    ret[:, 5, :] = -(128 - i)
    c["c_ret"] = ret
    pj = np.zeros((128, 2), np.float32)
    pj[:, 0] = -(127 - np.arange(128))
    pj[:, 1] = -np.arange(128)
    c["c_pj"] = pj
    qc = np.arange(64)[None, :]
    kc = np.arange(64)[:, None]
    cs = np.clip(qc - 8, 0, 48)
    ok = ((kc >= cs) & (kc < cs + 16)).astype(np.float32)
    na = np.zeros((128, 2, 64), np.float32)
    na[:, 0, :] = np.concatenate([ok, ok], 0) * 8.0
    na[:, 1, :] = (np.concatenate([ok, ok], 0) - 1.0) * (-NEGBIG)
    c["c_na"] = na
    return c


def _rpb_layout(rpb):
    kc = np.arange(64)[:, None]
    qc = np.arange(64)[None, :]
    d = kc - qc + 15
    valid = (d >= 0) & (d <= 30)
    dcl = np.clip(d, 0, 30)
    g = rpb[:, :, dcl]
    g = np.where(valid[None, None], g, np.float32(0)).astype(np.float32)
    g = g[:, ::-1]
    t = np.ascontiguousarray(g.transpose(2, 0, 1, 3))
    return np.ascontiguousarray(np.concatenate([t, t], 0))


def _na_plan():
    def row_start(r):
        return min(max(r - 4, 0), 24)
    plan = []
    cfgs = {}
    for t in range(16):
        lst = []
        for kt in range(16):
            blocks = []
            anyv = False
            for a in range(2):
                for b in range(2):
                    rq = 2 * t + b
                    rk = 2 * kt + a
                    v = row_start(rq) <= rk < row_start(rq) + 8
                    dr = rk - rq + 7
                    blocks.append((14 - dr) if v else None)
                    anyv = anyv or v
            if anyv:
                key = tuple(blocks)
                if key not in cfgs:
                    cfgs[key] = len(cfgs)
                lst.append((kt, cfgs[key]))
        plan.append(lst)
    return plan, cfgs


def build(dbg=(), stop_after=None, lite=False):
    nc = bass.Bass("TRN2", target_bir_lowering=False)
    dbg = set(dbg)

    def din(name, shape):
        return nc.dram_tensor(name, list(shape), F32, kind="ExternalInput").ap()

    x_d = din("x", [S, D])
    mem_d = din("mem", [MEM, D])
    g_mix_d = din("g_mix", [D])
    w_in_d = din("w_in", [D, 8192])
    decf_d = din("ret_decay_fwd", [4])
    decb_d = din("ret_decay_bwd", [4])
    gn_d = din("ret_norm_gain", [D])
    w_ro_d = din("w_ret_o", [D, D])
    rpbT_d = din("rpbT", [128, 8, 15, 64])
    w_nao_d = din("w_na_o", [512, D])
    g_mem_d = din("g_mem", [D])
    w_kv_d = din("w_mem_kv", [D, D])
    w_xao_d = din("w_xa_o", [512, D])
    w_out_d = din("w_out", [D, D])
    g_ffn_d = din("g_ffn", [D])
    w_rg_d = din("w_router_group", [D, 4])
    b_rg_d = din("b_router_group", [4])
    w_re_d = din("w_router_expert", [D, 16])
    b_re_d = din("b_router_expert", [16])
    if lite:
        w_eg_d = din("w_exp_gate", [1, 128, 512])
        w_eu_d = din("w_exp_up", [1, 128, 512])
        w_ed_d = din("w_exp_down", [1, 128, D])
    else:
        w_eg_d = din("w_exp_gate", [16, D, 512])
        w_eu_d = din("w_exp_up", [16, D, 512])
        w_ed_d = din("w_exp_down", [16, 512, D])
    g_fin_d = din("g_final", [D])
    w6_d = din("w6", [4, 128, 40 * 256])
    wret_d = din("wret", [4, 128, KC * 768])
    gnpk_d = din("gn_pk", [128, KC])
    c_ident_d = din("c_ident", [128, 128])
    c_rope_d = din("c_rope", [128, NT, 2, 64])
    c_ret_d = din("c_ret", [128, 6, 128])
    c_pj_d = din("c_pj", [128, 2])
    c_na_d = din("c_na", [128, 2, 64])
    out_d = nc.dram_tensor("out", [S, D], F32, kind="ExternalOutput").ap()

    na_plan, na_cfgs = _na_plan()
    NCFG = len(na_cfgs)

    es = ExitStack()
    with es:
        KB = 1024
        TOTAL = 196 * KB
        arena_t = es.enter_context(nc.sbuf_tensor("arena", [128, TOTAL // 4], F32))
        CONST = Arena(arena_t, 0, 20 * KB)
        HTR = Arena(arena_t, 20 * KB, 52 * KB)
        BIG = Arena(arena_t, 52 * KB, 116 * KB)
        WAR = Arena(arena_t, 116 * KB, 164 * KB)
        WORK = Arena(arena_t, 164 * KB, 196 * KB)

        pbanks = [es.enter_context(nc.psum_tensor(f"pb{i}", [128, 512], F32)) for i in range(8)]
        PB = [Buf(f"pb{i}") for i in range(8)]
        bank_i = [0]

        def bank():
            k = bank_i[0] % 8
            bank_i[0] += 1
            return pbanks[k], PB[k]

        def sem(name):
            return es.enter_context(nc.semaphore(name))

        PE = Eng("pe", nc.tensor, sem("s_pe"))
        ACT = Eng("act", nc.scalar, sem("s_act"))
        DVE = Eng("dve", nc.vector, sem("s_dve"))
        POOL = Eng("pool", nc.gpsimd, sem("s_pool"))
        SP = Eng("sp", nc.sync, sem("s_sp"))
        qsp = DmaQ(SP, [sem(f"dsp{i}") for i in range(16)])
        qpl = DmaQ(POOL, [sem(f"dpl{i}") for i in range(12)])
        es.enter_context(nc.Block())
        ENG = {"act": ACT, "dve": DVE, "pool": POOL}
        HND = {"act": nc.scalar, "dve": nc.vector, "pool": nc.gpsimd}

        def barrier():
            assert not PE.pending
            engs = [PE, ACT, DVE, POOL]
            for e in engs + [SP]:
                for o in engs:
                    if o is not e and o.count > 0:
                        e.wait(Tok(o.sem, o.count))

        def mm(out, lhsT, rhs, start, stop, R, W, sig=None):
            if sig is None:
                sig = stop
            return PE.op(lambda: nc.tensor.matmul(out, lhsT=lhsT, rhs=rhs, start=start, stop=stop), R, W, sig)

        def tr(out, in_, ident, R, W, sig):
            return PE.op(lambda: nc.tensor.transpose(out=out, in_=in_, identity=ident), R, W, sig)

        def act(out, in_, func, R, W, **kw):
            return ACT.op(lambda: nc.scalar.activation(out=out, in_=in_, func=func, **kw), R, W)

        def tt(e, out, in0, in1, op, R, W):
            return ENG[e].op(lambda: HND[e].tensor_tensor(out=out, in0=in0, in1=in1, op=op), R, W)

        def ts(e, out, in0, s1, s2, op0, op1, R, W):
            if op1 is None:
                return ENG[e].op(lambda: HND[e].tensor_scalar(out=out, in0=in0, scalar1=s1, scalar2=None, op0=op0), R, W)
            return ENG[e].op(lambda: HND[e].tensor_scalar(out=out, in0=in0, scalar1=s1, scalar2=s2, op0=op0, op1=op1), R, W)

        def stt(out, in0, scalar, in1, op0, op1, R, W):
            return DVE.op(lambda: nc.vector.scalar_tensor_tensor(out=out, in0=in0, scalar=scalar, in1=in1, op0=op0, op1=op1), R, W)

        def cp(e, out, in_, R, W):
            if e == "act":
                return ACT.op(lambda: nc.scalar.copy(out=out, in_=in_), R, W)
            return ENG[e].op(lambda: HND[e].tensor_copy(out=out, in_=in_), R, W)

        def red(out, in_, op, R, W):
            return DVE.op(lambda: nc.vector.tensor_reduce(out=out, in_=in_, axis=AX.X, op=op), R, W)

        dumps = []

        def dump(name, ap, R, shape):
            if name not in dbg:
                return
            dt = nc.dram_tensor("dbg_" + name, list(shape), ap.dtype, kind="ExternalOutput").ap()
            dumps.append(qsp.dma(dt, ap, reads=R))

        ident_f = CONST.alloc([128], F32)
        ident_b = CONST.alloc([128], BF16)
        ones_b = CONST.alloc([128], BF16)
        neg_b = CONST.alloc([128], BF16)
        gA = CONST.alloc([D], F32)
        gB = CONST.alloc([D], F32)
        WT = CONST.alloc([4, 128], F32)
        gfq = CONST.alloc([4, 128], F32)
        gbq = CONST.alloc([4, 128], F32)
        kdec = CONST.alloc([4, 2], F32)
        lg = CONST.alloc([2, 4], F32)
        agam = CONST.alloc([2, 4], F32)
        pj = CONST.alloc([2], F32)
        small = CONST.alloc([64], F32)
        B_ident = Buf("ident")
        B_gA = Buf("gA")
        B_gB = Buf("gB")
        B_ret = Buf("retc")
        B_small = Buf("small")
        B_ss = [Buf(f"ss{i}") for i in range(24)]

        qsp.dma(ident_f, c_ident_d[:, :], writes=[B_ident])
        cp("dve", ident_b, ident_f, [B_ident], [B_ident])
        POOL.op(lambda: nc.gpsimd.memset(ones_b, 1.0), [], [B_ident])
        POOL.op(lambda: nc.gpsimd.memset(neg_b, NEGBIG), [], [B_ident])
        qsp.dma(gA, g_mix_d.partition_broadcast(128), writes=[B_gA])
        m0 = WORK.mark()

        cret = CONST.alloc([6, 128], F32)
        B_cret = Buf("cret")
        tmpb = gB.rearrange("p (a b) -> p a b", a=8)[:, 0:4, :]

        def emit_ret_consts_act():
            qsp.dma(pj, c_pj_d[:, :], writes=[B_ret])
            qsp.dma(lg[:, 0, :], decf_d.partition_broadcast(128), writes=[B_ret])
            qsp.dma(lg[:, 1, :], decb_d.partition_broadcast(128), writes=[B_ret])
            qsp.dma(cret, c_ret_d[:, :, :], writes=[B_cret])
            lgf = lg[:].rearrange("p a b -> p (a b)")
            act(lgf, lgf, AF.Exp, [B_ret], [B_ret], scale=-1.0)
            act(lgf, lgf, AF.Ln, [B_ret], [B_ret], bias=1.0)
            act(agam[:].rearrange("p a b -> p (a b)"), lgf, AF.Exp, [B_ret], [B_ret], scale=-128.0)
            for h in range(4):
                lf = lg[:, 0, h:h + 1]
                lb = lg[:, 1, h:h + 1]
                act(WT[:, h, :], cret[:, 0, :], AF.Exp, [B_cret, B_ret], [B_ret], scale=lf)
                act(tmpb[:, h, :], cret[:, 2, :], AF.Exp, [B_cret, B_ret], [B_gB], scale=lb)
                act(gfq[:, h, :], cret[:, 4, :], AF.Exp, [B_cret, B_ret], [B_ret], scale=lf)
                act(gbq[:, h, :], cret[:, 5, :], AF.Exp, [B_cret, B_ret], [B_ret], scale=lb)
                act(kdec[:, h, 0:1], pj[:, 0:1], AF.Exp, [B_ret], [B_ret], scale=lf)
                act(kdec[:, h, 1:2], pj[:, 1:2], AF.Exp, [B_ret], [B_ret], scale=lb)

        def emit_ret_consts_dve():
            for h in range(4):
                stt(WT[:, h, :], WT[:, h, :], QSCALE, cret[:, 1, :], ALU.mult, ALU.mult, [B_ret, B_cret], [B_ret])
                stt(tmpb[:, h, :], tmpb[:, h, :], QSCALE, cret[:, 3, :], ALU.mult, ALU.mult, [B_gB, B_cret], [B_gB])
                tt("dve", WT[:, h, :], WT[:, h, :], tmpb[:, h, :], ALU.add, [B_ret, B_gB], [B_ret])
            gq = gfq[:].rearrange("p a b -> p (a b)")
            ts("dve", gq, gq, QSCALE, None, ALU.mult, None, [B_ret], [B_ret])
            gq2 = gbq[:].rearrange("p a b -> p (a b)")
            ts("dve", gq2, gq2, QSCALE, None, ALU.mult, None, [B_ret], [B_ret])

        hT = HTR.alloc([KC, S], BF16)
        B_hT = [Buf(f"hT{i}") for i in range(4)]

        def rms_to_fm(src_d, ntiles, gtile, B_g, dstT, B_dst_of, tag):
            m = WORK.mark()
            NX = 3
            xs = [WORK.alloc([D], F32) for _ in range(NX)]
            B_xs = [Buf(f"{tag}xs{i}") for i in range(NX)]
            hb = [WORK.alloc([D], BF16) for _ in range(2)]
            B_hb = [Buf(f"{tag}hb{i}") for i in range(2)]
            junk = WORK.alloc([D], BF16)
            B_junk = Buf(tag + "junk")
            for i in range(min(NX - 1, ntiles)):
                qsp.dma(xs[i % NX], src_d[i * 128:(i + 1) * 128, :], writes=[B_xs[i % NX]])
            def stage_a(i):
                xt, Bx = xs[i % NX], B_xs[i % NX]
                if i + NX - 1 < ntiles:
                    j = i + NX - 1
                    qsp.dma(xs[j % NX], src_d[j * 128:(j + 1) * 128, :], writes=[B_xs[j % NX]])
                ss = small[:, (i % 8):(i % 8) + 1]
                Bs = B_ss[i % 8]
                act(junk, xt, AF.Square, [Bx], [B_junk, Bs], accum_out=ss)
                act(ss, ss, AF.Sqrt, [Bs], [Bs], scale=1.0 / D, bias=1e-6)
                DVE.op(lambda: nc.vector.reciprocal(out=ss, in_=ss), [Bs], [Bs])
                stt(hb[i % 2], xt, ss, gtile, ALU.mult, ALU.mult, [Bx, Bs, B_g], [B_hb[i % 2]])

            stage_a(0)
            for i in range(ntiles):
                if i + 1 < ntiles:
                    stage_a(i + 1)
                pb, Bp = bank()
                pT = pb[:].bitcast(BF16)
                for k in range(KC):
                    tr(pT[:, k * 128:(k + 1) * 128], hb[i % 2][:, k * 128:(k + 1) * 128], ident_b, [B_hb[i % 2], B_ident], [Bp], k == KC - 1)
                cp("act" if i % 2 == 0 else "dve", dstT[:, :, i * 128:(i + 1) * 128],
                   pT.rearrange("p (k t) -> p k t", k=KC), [Bp], [B_dst_of(i)])
            barrier()
            WORK.release(m)

        w_in_v = w_in_d.rearrange("(k p) f -> p k f", p=128)
        ret_w = WAR.alloc([KC, 768], BF16)
        B_retw = Buf("retw")

        def ret_load_w(h):
            qpl.dma(ret_w[:].rearrange("p k f -> p (k f)"), wret_d[h], writes=[B_retw], max_dma_last_dim=8192)

        ret_load_w(0)
        rms_to_fm(x_d, NT, gA, B_gA, hT, lambda i: B_hT[i // 4], "x")
        dump("hT", hT, B_hT, [128, KC, S])
        if stop_after == "h":
            return _finish(nc, SP, dumps)

        zT = BIG.alloc([8, S], BF16)
        oT = BIG.alloc([4, S], BF16)
        xaT = BIG.alloc([4, S], BF16)
        B_zT = Buf("zT")
        B_oT = Buf("oT")
        B_xaT = Buf("xaT")

        def retention():
            mW = WAR.lo
            X1 = Arena(arena_t, WAR.top, WAR.hi)
            X2 = Arena(arena_t, BIG.lo + 32 * KB, BIG.hi)
            rope = X2.alloc([NT, 2, 64], F32)
            v_h = X2.alloc([NT, 256], BF16)
            sg = X2.alloc([NT, 256], BF16)
            qkr = X2.alloc([NT, 2, 128], BF16)
            stf = X1.alloc([NT, 256], BF16)
            stb = X1.alloc([NT, 256], BF16)
            qT = X1.alloc([S], BF16)
            kT = X1.alloc([S], BF16)
            qkfb = [X1.alloc([4, 256], F32) for _ in range(2)]
            rt12 = X1.alloc([2, 4, 2, 64], F32)
            scr = WORK.alloc([8 * KB // 4], F32)
            qdf = WORK.alloc([NT, 128], BF16)
            qdb = WORK.alloc([NT, 128], BF16)
            kdf = WORK.alloc([NT, 128], BF16)
            kdb = WORK.alloc([NT, 128], BF16)
            Sst = WORK.alloc([2, 2, 256], F32)
            PTb = [WORK.alloc([128], BF16) for _ in range(4)]
            junkr = WORK.alloc([256], BF16)
            mv_ = WORK.alloc([8, 4], F32)
            gnb = gB
            t1 = rt12[:, 0]
            t2 = rt12[:, 1]
            Yh = scr[:, 0:2048].rearrange("p (t c) -> p t c", t=8)
            z = qkr[:].rearrange("p t a c -> p t (a c)")
            B = {n: Buf(n) for n in ["rope", "v", "sg", "qkr", "stf", "stb", "qT", "kT", "scr", "qdf", "qdb", "kdf", "kdb",
                                      "junkr", "mv", "PT0", "PT1", "PT2", "PT3", "qkf0", "qkf1", "t12",
                                      "S00", "S01", "S10", "S11", "Yh0", "Yh1", "mv0", "mv1"]}
            qsp.dma(rope, c_rope_d[:, :, :, :], writes=[B["rope"]])

            def z_transposes(hz):
                for e2 in range(2):
                    for half in range(2):
                        pb, Bp = bank()
                        pT = pb[:].bitcast(BF16)
                        for ii in range(8):
                            i = half * 8 + ii
                            tr(pT[:, ii * 128:(ii + 1) * 128], z[:, i, e2 * 128:(e2 + 1) * 128], ident_b, [B["qkr"], B_ident], [Bp], ii == 7)
                        cp("act" if half == 0 else "dve", zT[:, 2 * hz + e2, half * 1024:(half + 1) * 1024], pT, [Bp], [B_zT])

            for h in range(4):
                sl, Bw = ret_w, B_retw
                for i in range(NT):
                    pA, BA = bank()
                    pBk, BBk = bank()
                    for k in range(KC):
                        mm(pA[:, 0:512], hT[:, k, i * 128:(i + 1) * 128], sl[:, k, 0:512], k == 0, k == KC - 1, [B_hT[i // 4], Bw], [BA])
                    for k in range(KC):
                        mm(pBk[:, 0:256], hT[:, k, i * 128:(i + 1) * 128], sl[:, k, 512:768], k == 0, k == KC - 1, [B_hT[i // 4], Bw], [BBk])
                    if i == 1 and h > 0:
                        z_transposes(h - 1)
                    if i == 0 and h == 0:
                        emit_ret_consts_act()
                    qkf, Bqkf = qkfb[(i // 4) % 2], B[f"qkf{(i // 4) % 2}"]
                    cp("act", qkf[:, i % 4, :], pA[:, 0:256], [BA], [Bqkf])
                    cp("act", v_h[:, i, :], pA[:, 256:512], [BA], [B["v"]])
                    act(sg[:, i, :], pBk[:, 0:256], AF.Silu, [BBk], [B["sg"]])
                    if i % 4 == 3:
                        i0 = i - 3
                        src = qkf.rearrange("p t (a b c) -> p t a b c", a=2, b=2)
                        a1 = src[:, :, :, 0, :]
                        a2 = src[:, :, :, 1, :]
                        cosb = rope[:, i0:i0 + 4, 0, :].unsqueeze(2).broadcast_to([128, 4, 2, 64])
                        sinb = rope[:, i0:i0 + 4, 1, :].unsqueeze(2).broadcast_to([128, 4, 2, 64])
                        dst = qkr[:, i0:i0 + 4, :, :].rearrange("p t a (b c) -> p t a b c", b=2)
                        R0 = [Bqkf, B["rope"]]
                        tt("dve", t1, a1, cosb, ALU.mult, R0, [B["t12"]])
                        tt("dve", t2, a2, sinb, ALU.mult, R0, [B["t12"]])
                        tt("dve", dst[:, :, :, 0, :], t1, t2, ALU.subtract, [B["t12"]], [B["qkr"]])
                        tt("dve", t1, a1, sinb, ALU.mult, R0, [B["t12"]])
                        tt("dve", t2, a2, cosb, ALU.mult, R0, [B["t12"]])
                        tt("dve", dst[:, :, :, 1, :], t1, t2, ALU.add, [B["t12"]], [B["qkr"]])
                        act(kdf[:, i0:i0 + 4, :], qkr[:, i0:i0 + 4, 1, :], AF.Identity, [B["qkr"], B_ret], [B["kdf"]], scale=kdec[:, h, 0:1])
                        act(kdb[:, i0:i0 + 4, :], qkr[:, i0:i0 + 4, 1, :], AF.Identity, [B["qkr"], B_ret], [B["kdb"]], scale=kdec[:, h, 1:2])
                if h + 1 < 4:
                    ret_load_w(h + 1)
                if h == 0:
                    emit_ret_consts_dve()
                if stop_after == "ret_proj":
                    dump("qkr", qkr, [B["qkr"]], [128, NT, 2, 128])
                    return True

                def qk_transpose_group(g):
                    a, half = g // 2, g % 2
                    dstT, Bd = (qT, B["qT"]) if a == 0 else (kT, B["kT"])
                    pb, Bp = bank()
                    pT = pb[:].bitcast(BF16)
                    for ii in range(8):
                        i = half * 8 + ii
                        tr(pT[:, ii * 128:(ii + 1) * 128], qkr[:, i, a, :], ident_b, [B["qkr"], B_ident], [Bp], ii == 7)
                    cp("act" if half == 0 else "dve", dstT[:, half * 1024:(half + 1) * 1024], pT, [Bp], [Bd])

                DVE.op(lambda: nc.vector.memset(Sst[:, 1, 0, :], 0.0), [], [B["S10"]])
                DVE.op(lambda: nc.vector.memset(Sst[:, 1, 1, :], 0.0), [], [B["S11"]])
                if h == 0:
                    POOL.op(lambda: nc.gpsimd.memset(stf[:, 0, :], 0.0), [], [B["stf"]])
                    POOL.op(lambda: nc.gpsimd.memset(stb[:, NT - 1, :], 0.0), [], [B["stb"]])
                for n in range(NT - 1):
                    nb = NT - 1 - n
                    pk, Bk = bank()
                    mm(pk[:, 0:256], kdf[:, n, :], v_h[:, n, :], True, True, [B["kdf"], B["v"]], [Bk], sig=False)
                    mm(pk[:, 256:512], kdb[:, nb, :], v_h[:, nb, :], True, True, [B["kdb"], B["v"]], [Bk], sig=True)
                    po, pn = (n + 1) % 2, n % 2
                    stt(Sst[:, pn, 0, :], Sst[:, po, 0, :], agam[:, 0, h:h + 1], pk[:, 0:256], ALU.mult, ALU.add,
                        [B[f"S{po}0"], Bk, B_ret], [B[f"S{pn}0"]])
                    stt(Sst[:, pn, 1, :], Sst[:, po, 1, :], agam[:, 1, h:h + 1], pk[:, 256:512], ALU.mult, ALU.add,
                        [B[f"S{po}1"], Bk, B_ret], [B[f"S{pn}1"]])
                    cp("act", stf[:, n + 1, :], Sst[:, pn, 0, :], [B[f"S{pn}0"]], [B["stf"]])
                    cp("act", stb[:, nb - 1, :], Sst[:, pn, 1, :], [B[f"S{pn}1"]], [B["stb"]])
                    if n in (0, 3, 6, 9):
                        qk_transpose_group(n // 3)
                qTv = qT.rearrange("p (n c) -> p n c", n=NT)
                tt("dve", qdf[:], qTv, gfq[:, h, :].unsqueeze(1).broadcast_to([128, NT, 128]), ALU.mult, [B["qT"], B_ret], [B["qdf"]])
                tt("dve", qdb[:], qTv, gbq[:, h, :].unsqueeze(1).broadcast_to([128, NT, 128]), ALU.mult, [B["qT"], B_ret], [B["qdb"]])
                if stop_after == "ret_state":
                    dump("stf", stf, [B["stf"]], [128, NT, 256])
                    return True
                def scores(n):
                    pS, BS = bank()
                    cs = slice(n * 128, (n + 1) * 128)
                    mm(pS[:, 0:128], kT[:, cs], qT[:, cs], True, True, [B["kT"], B["qT"]], [BS])
                    tt("dve", PTb[n % 4], pS[:, 0:128], WT[:, h, :], ALU.mult, [BS, B_ret], [B[f"PT{n % 4}"]])

                def gn_batch(b):
                    hb_ = b % 2
                    n0 = 4 * b
                    Yq = Yh[:, hb_ * 4:(hb_ + 1) * 4, :]
                    Bq, Bmv = B[f"Yh{hb_}"], B[f"mv{hb_}"]
                    mvq = mv_[:, hb_ * 4:(hb_ + 1) * 4, :]
                    msum = mvq[:, :, 0]
                    vsum = mvq[:, :, 1]
                    ts("dve", msum, msum, 1.0 / 256, None, ALU.mult, None, [Bmv], [Bmv])
                    mb_ = msum.unsqueeze(2).broadcast_to([128, 4, 256])
                    tt("dve", Yq, Yq, mb_, ALU.subtract, [Bq, Bmv], [Bq])
                    for j in range(4):
                        act(junkr, Yq[:, j, :], AF.Square, [Bq], [B["junkr"], Bmv], accum_out=mvq[:, j, 1:2])
                    act(vsum, vsum, AF.Sqrt, [Bmv], [Bmv], scale=1.0 / 256, bias=1e-5)
                    DVE.op(lambda: nc.vector.reciprocal(out=vsum, in_=vsum), [Bmv], [Bmv])
                    rb_ = vsum.unsqueeze(2).broadcast_to([128, 4, 256])
                    tt("dve", Yq, Yq, rb_, ALU.mult, [Bq, Bmv], [Bq])
                    tt("dve", z[:, n0:n0 + 4, :], Yq, sg[:, n0:n0 + 4, :], ALU.mult, [Bq, B["sg"]], [B["qkr"]])

                LOOK = 3
                for n in range(LOOK):
                    scores(n)
                for n in range(NT):
                    if n + LOOK < NT:
                        scores(n + LOOK)
                    PT, BPT = PTb[n % 4], B[f"PT{n % 4}"]
                    pY, BY = bank()
                    mm(pY[:, 0:256], PT, v_h[:, n, :], True, False, [BPT, B["v"]], [BY])
                    mm(pY[:, 0:256], qdf[:, n, :], stf[:, n, :], False, False, [B["qdf"], B["stf"]], [BY])
                    mm(pY[:, 0:256], qdb[:, n, :], stb[:, n, :], False, True, [B["qdb"], B["stb"]], [BY])
                    hb_ = (n // 4) % 2
                    Yq = Yh[:, hb_ * 4:(hb_ + 1) * 4, :]
                    mvq = mv_[:, hb_ * 4:(hb_ + 1) * 4, :]
                    act(Yq[:, n % 4, :], pY[:, 0:256], AF.Identity, [BY], [B[f"Yh{hb_}"], B[f"mv{hb_}"]], accum_out=mvq[:, n % 4, 0:1])
                    if n % 4 == 3 and n // 4 >= 1:
                        gn_batch(n // 4 - 1)
                gn_batch(3)
                if h == 3:
                    z_transposes(h)
                if stop_after == "ret_h0":
                    return True
            barrier()
            WAR.release(mW)
            WORK.release(m0)

        if retention():
            return _finish(nc, SP, dumps)
        dump("zT", zT, [B_zT], [128, 8, S])
        if stop_after == "ret":
            return _finish(nc, SP, dumps)

        def natten():
            mW = WAR.mark()
            wv = WAR.alloc([KC, 512], BF16)
            wqk = [WAR.alloc([KC, 256], BF16) for _ in range(2)]
            nva = WAR.alloc([NT, 8, 65], BF16)
            masks = WAR.alloc([2, NCFG, 128], BF16)
            X2 = Arena(arena_t, BIG.lo + 48 * KB, BIG.hi)
            nqT = X2.alloc([S], BF16)
            nkz = X2.alloc([2, S], BF16)
            otm = X2.alloc([NT, 128], BF16)
            Hf = WORK.alloc([2, 15, 64], F32)
            cna = WORK.alloc([2, 64], F32)
            PTn = [WORK.alloc([5, 128], BF16) for _ in range(3)]
            B_mask = [[[Buf(f"mk{hh}_{ci}_{a}") for a in range(2)] for ci in range(NCFG)] for hh in range(2)]
            rc = WORK.alloc([2], F32)
            B = {n: Buf(n) for n in ["wv", "wqk0", "wqk1", "nva", "masks", "nqT", "nkT", "otm", "Hf", "cna", "PT0", "PT1", "PT2", "rc"]}
            qpl.dma(wv, w_in_v[:, :, 4096:4608], writes=[B["wv"]])
            qsp.dma(cna, c_na_d[:, :, :], writes=[B["cna"]])
            POOL.op(lambda: nc.gpsimd.memset(nva[:, :, :, 64:65], 1.0), [], [B["nva"]])
            POOL.op(lambda: nc.gpsimd.memset(nkz[64:128, 0, :], 0.0), [], [B["nkT"]])
            POOL.op(lambda: nc.gpsimd.memset(nkz[0:64, 1, :], 0.0), [], [B["nkT"]])

            def load_qk(c):
                sl, Bw = wqk[c % 2], B[f"wqk{c % 2}"]
                qpl.dma(sl[:, :, 0:128], w_in_v[:, :, 3072 + c * 128:3072 + (c + 1) * 128], writes=[Bw])
                qpl.dma(sl[:, :, 128:256], w_in_v[:, :, 3584 + c * 128:3584 + (c + 1) * 128], writes=[Bw])

            load_qk(0)
            for i in range(NT):
                pb, Bp = bank()
                for k in range(KC):
                    mm(pb[:, 0:512], hT[:, k, i * 128:(i + 1) * 128], wv[:, k, :], k == 0, k == KC - 1, [B_hT[i // 4], B["wv"]], [Bp])
                cp("act" if i % 2 == 0 else "dve", nva[:, i, :, 0:64], pb[:, 0:512].rearrange("p (h d) -> p h d", h=8), [Bp], [B["nva"]])
            for c in range(4):
                if c + 1 < 4:
                    load_qk(c + 1)
                sl, Bw = wqk[c % 2], B[f"wqk{c % 2}"]
                for a in range(2):
                    for tb in range(4):
                        pb, Bp = bank()
                        tsl = slice(tb * 512, (tb + 1) * 512)
                        for k in range(KC):
                            mm(pb[:, 0:512], sl[:, k, a * 128:(a + 1) * 128], hT[:, k, tsl], k == 0, k == KC - 1,
                               [Bw, B_hT[tb]], [Bp])
                        if a == 0:
                            cp("act" if tb % 2 == 0 else "dve", nqT[:, tsl], pb[:, 0:512], [Bp], [B["nqT"]])
                        else:
                            cp("act", nkz[0:64, 0, tsl], pb[0:64, 0:512], [Bp], [B["nkT"]])
                            cp("dve", nkz[64:128, 1, tsl], pb[64:128, 0:512], [Bp], [B["nkT"]])
                qsp.dma(Hf, rpbT_d[:, 2 * c:2 * c + 2, :, :], writes=[B["Hf"]])
                Hv = Hf[:].rearrange("p h r q -> p (h r) q")
                tt("dve", Hv, Hv, cna[:, 0, :].unsqueeze(1).broadcast_to([128, 30, 64]), ALU.mult, [B["Hf"], B["cna"]], [B["Hf"]])
                tt("dve", Hv, Hv, cna[:, 1, :].unsqueeze(1).broadcast_to([128, 30, 64]), ALU.add, [B["Hf"], B["cna"]], [B["Hf"]])
                cnt = 0
                for hh in range(2):
                    for key, ci in na_cfgs.items():
                        Bm = B_mask[hh][ci]
                        for a in range(2):
                            ps_ = slice(a * 64, (a + 1) * 64)
                            d0, d1 = key[2 * a], key[2 * a + 1]
                            e = ["dve", "pool", "act"][cnt % 3]
                            cnt += 1
                            if d0 is not None and d1 is not None:
                                assert d1 == d0 + 1
                                cp(e, masks[ps_, hh, ci, :], Hf[ps_, hh, d0:d0 + 2, :].rearrange("p r q -> p (r q)"), [B["Hf"]], [Bm[a]])
                            else:
                                for b_, dd in enumerate((d0, d1)):
                                    fs = slice(b_ * 64, (b_ + 1) * 64)
                                    if dd is None:
                                        cp(e, masks[ps_, hh, ci, fs], neg_b[ps_, 0:64], [B_ident], [Bm[a]])
                                    else:
                                        cp(e, masks[ps_, hh, ci, fs], Hf[ps_, hh, dd, :], [B["Hf"]], [Bm[a]])
                iters = [(t, hh) for t in range(NT) for hh in range(2)]
                pOs = {}

                def stageA(j):
                    t, hh = iters[j]
                    lst = na_plan[t]
                    PT, BPT = PTn[j % 3], B[f"PT{j % 3}"]
                    p1, B1 = bank()
                    p2, B2 = (None, None)
                    if len(lst) > 4:
                        p2, B2 = bank()
                    for idx, (kt, ci) in enumerate(lst):
                        pp, Bpp = (p1, B1) if idx < 4 else (p2, B2)
                        o_ = pp[:, (idx % 4) * 128:(idx % 4 + 1) * 128]
                        mm(o_, nkz[:, hh, kt * 128:(kt + 1) * 128], nqT[:, t * 128:(t + 1) * 128], True, False, [B["nkT"], B["nqT"]], [Bpp], sig=False)
                        last = (idx == min(len(lst), 4) - 1) or (idx == len(lst) - 1)
                        mm(o_, ident_b, masks[:, hh, ci, :], False, True, [B_ident] + B_mask[hh][ci], [Bpp], sig=last)
                    n1 = min(len(lst), 4)
                    act(PT[:, 0:n1, :], p1[:, 0:n1 * 128].rearrange("p (s q) -> p s q", s=n1), AF.Exp, [B1], [BPT], scale=0.125)
                    if len(lst) > 4:
                        act(PT[:, 4, :], p2[:, 0:128], AF.Exp, [B2], [BPT], scale=0.125)

                def stageB(j):
                    t, hh = iters[j]
                    lst = na_plan[t]
                    PT, BPT = PTn[j % 3], B[f"PT{j % 3}"]
                    if hh == 0:
                        pOs[t] = bank()
                    pO, BO = pOs[t]
                    for idx, (kt, ci) in enumerate(lst):
                        mm(pO[:, hh * 65:(hh + 1) * 65], PT[:, idx, :], nva[:, kt, 2 * c + hh, :], idx == 0, idx == len(lst) - 1,
                           [BPT, B["nva"]], [BO])
                    if hh == 1:
                        pOv = pO[:, 0:130].rearrange("p (h d) -> p h d", h=2)
                        DVE.op(lambda: nc.vector.reciprocal(out=rc[:].unsqueeze(2), in_=pOv[:, :, 64:65]), [BO], [B["rc"]])
                        tt("dve", otm[:, t, :].rearrange("p (h d) -> p h d", h=2), pOv[:, :, 0:64],
                           rc[:].unsqueeze(2).broadcast_to([128, 2, 64]), ALU.mult, [BO, B["rc"]], [B["otm"]])

                LOOKN = 2
                for j in range(min(LOOKN, len(iters))):
                    stageA(j)
                for j in range(len(iters)):
                    if j + LOOKN < len(iters):
                        stageA(j + LOOKN)
                    stageB(j)
                for half in range(2):
                    pb, Bp = bank()
                    pT = pb[:].bitcast(BF16)
                    for ii in range(8):
                        i = half * 8 + ii
                        tr(pT[:, ii * 128:(ii + 1) * 128], otm[:, i, :], ident_b, [B["otm"], B_ident], [Bp], ii == 7)
                    cp("act" if half == 0 else "dve", oT[:, c, half * 1024:(half + 1) * 1024], pT, [Bp], [B_oT])
            barrier()
            WAR.release(mW)
            WORK.release(m0)

        natten()
        dump("oT", oT, [B_oT], [128, 4, S])
        if stop_after == "na":
            return _finish(nc, SP, dumps)

        A6 = Arena(arena_t, WAR.lo, WORK.hi)
        mT = A6.alloc([KC, S], BF16)
        wg6_1 = A6.alloc([40, 256], BF16)
        sgt = [A6.alloc([512], F32) for _ in range(2)]
        mtmp = [A6.alloc([512], F32) for _ in range(2)]
        wg6_0 = A6.alloc([40, 256], BF16)
        wg6 = [wg6_0, wg6_1]
        B_w6 = [Buf("w6a"), Buf("w6b")]
        gnpk = CONST.alloc([KC], F32)
        B_gnpk = Buf("gnpk")
        qsp.dma(gnpk, gnpk_d[:, :], writes=[B_gnpk])

        def load_w6(cp_):
            sl, Bw = wg6[cp_ % 2], B_w6[cp_ % 2]
            slf = sl[:].rearrange("p a b -> p (a b)")
            for q_ in range(2):
                qpl.dma(slf[:, q_ * 5120:(q_ + 1) * 5120], w6_d[cp_][:, q_ * 5120:(q_ + 1) * 5120], writes=[Bw], max_dma_last_dim=8192)

        def scale_w6(cp_):
            sl, Bw = wg6[cp_ % 2], B_w6[cp_ % 2]
            for k in range(KC):
                act(sl[:, k, :], sl[:, k, :], AF.Identity, [Bw, B_gnpk], [Bw], scale=gnpk[:, k:k + 1])

        def xattn():
            mW = WAR.mark()
            wkv = WAR.alloc([KC, D], BF16)
            wxq = WAR.alloc([KC, 512], BF16)
            xqT = [WAR.alloc([S], BF16) for _ in range(2)]
            memT = WAR.alloc([KC, MEM], BF16)
            mkT = WAR.alloc([4, MEM], BF16)
            mvv = WAR.alloc([2, 512], BF16)
            B = {n: Buf(n) for n in ["wkv", "wxq", "xq0", "xq1", "memT", "mkT", "mv", "PT0", "PT1", "rc0", "rc1"]}
            qpl.dma(wkv[:, :, 0:512], w_kv_d.rearrange("(k p) f -> p k f", p=128)[:, :, 0:512], writes=[B["wkv"]])
            qpl.dma(wkv[:, :, 512:1024], w_kv_d.rearrange("(k p) f -> p k f", p=128)[:, :, 512:1024], writes=[B["wkv"]])
            qpl.dma(wxq, w_in_v[:, :, 4608:5120], writes=[B["wxq"]])
            qsp.dma(gA, g_mem_d.partition_broadcast(128), writes=[B_gA])
            rms_to_fm(mem_d, 2, gA, B_gA, memT, lambda i: B["memT"], "m")
            load_w6(0)
            PTx = [WORK.alloc([2, 512], BF16) for _ in range(2)]
            rcx = [WORK.alloc([512], F32) for _ in range(2)]
            for h in range(4):
                pb, Bp = bank()
                for k in range(KC):
                    mm(pb[:, 0:256], wkv[:, k, h * 128:(h + 1) * 128], memT[:, k, :], k == 0, k == KC - 1, [B["wkv"], B["memT"]], [Bp])
                cp("act", mkT[:, h, :], pb[:, 0:256], [Bp], [B["mkT"]])
            for mt in range(2):
                pb, Bp = bank()
                for k in range(KC):
                    mm(pb[:, 0:512], memT[:, k, mt * 128:(mt + 1) * 128], wkv[:, k, 512:1024], k == 0, k == KC - 1, [B["wkv"], B["memT"]], [Bp])
                cp("dve", mvv[:, mt, :], pb[:, 0:512], [Bp], [B["mv"]])
            stepsx = [(h, tb) for h in range(4) for tb in range(4)]

            def xq_proj(h):
                xq, Bxq = xqT[h % 2], B[f"xq{h % 2}"]
                for tb in range(4):
                    pb, Bp = bank()
                    for k in range(KC):
                        mm(pb[:, 0:512], wxq[:, k, h * 128:(h + 1) * 128], hT[:, k, tb * 512:(tb + 1) * 512], k == 0, k == KC - 1,
                           [B["wxq"], B_hT[tb]], [Bp])
                    cp("act", xq[:, tb * 512:(tb + 1) * 512], pb[:, 0:512], [Bp], [Bxq])

            def xa_scores(j):
                h, tb = stepsx[j]
                if tb == 0:
                    xq_proj(h)
                xq, Bxq = xqT[h % 2], B[f"xq{h % 2}"]
                PT, BPT = PTx[j % 2], B[f"PT{j % 2}"]
                tsl = slice(tb * 512, (tb + 1) * 512)
                for mt in range(2):
                    pS, BS = bank()
                    mm(pS[:, 0:512], mkT[:, h, mt * 128:(mt + 1) * 128], xq[:, tsl], True, True, [B["mkT"], Bxq], [BS])
                    act(PT[:, mt, :], pS[:, 0:512], AF.Exp, [BS], [BPT], scale=QSCALE)

            def xa_pv(j):
                h, tb = stepsx[j]
                PT, BPT = PTx[j % 2], B[f"PT{j % 2}"]
                rcb, Brc = rcx[j % 2], B[f"rc{j % 2}"]
                tsl = slice(tb * 512, (tb + 1) * 512)
                pN, BN = bank()
                pD, BD = bank()
                for mt in range(2):
                    mm(pN[:, 0:512], mvv[:, mt, h * 128:(h + 1) * 128], PT[:, mt, :], mt == 0, mt == 1, [B["mv"], BPT], [BN])
                for mt in range(2):
                    mm(pD[:, 0:512], ones_b, PT[:, mt, :], mt == 0, mt == 1, [B_ident, BPT], [BD])
                DVE.op(lambda: nc.vector.reciprocal(out=rcb, in_=pD[:, 0:512]), [BD], [Brc])
                tt("dve", xaT[:, h, tsl], pN[:, 0:512], rcb, ALU.mult, [BN, Brc], [B_xaT])

            xa_scores(0)
            for j in range(len(stepsx)):
                if j + 1 < len(stepsx):
                    xa_scores(j + 1)
                xa_pv(j)
            barrier()
            WAR.release(mW)
            WORK.release(m0)

        xattn()
        dump("xaT", xaT, [B_xaT], [128, 4, S])
        if stop_after == "xa":
            return _finish(nc, SP, dumps)

        mW6 = WAR.mark()
        B_mT = [Buf(f"mT{i}") for i in range(4)]
        B_sg = [Buf("sg0"), Buf("sg1")]
        B_mt = [Buf("mt0"), Buf("mt1")]
        srcs = [(zT, B_zT, 8, 0), (oT, B_oT, 4, 8), (xaT, B_xaT, 4, 12)]
        it = 0
        wout = wg6[0][:].rearrange("p a b -> p (a b)")[:, 0:KC * D].rearrange("p (k f) -> p k f", k=KC)
        B_wout = B_w6[0]
        w_out_v = w_out_d.rearrange("(k p) f -> p k f", p=128)
        scale_w6(0)
        for c in range(KC):
            if c % 2 == 0 and c // 2 + 1 < KC // 2:
                load_w6(c // 2 + 1)
            if c % 2 == 1 and c // 2 + 1 < KC // 2:
                scale_w6(c // 2 + 1)
            if c == 6:
                qpl.dma(wout[:, :, 0:512], w_out_v[:, :, 0:512], writes=[B_wout])
                qpl.dma(wout[:, :, 512:1024], w_out_v[:, :, 512:1024], writes=[B_wout])
            sl, Bw = wg6[(c // 2) % 2], B_w6[(c // 2) % 2]
            co = (c % 2) * 128
            for tb in range(4):
                tsl = slice(tb * 512, (tb + 1) * 512)
                for b_, (src, Bsrc, nk, woff) in enumerate(srcs):
                    pY, BY = bank()
                    pG, BG = bank()
                    for k in range(nk):
                        mm(pY[:, 0:512], sl[:, woff + k, co:co + 128], src[:, k, tsl], k == 0, k == nk - 1, [Bw, Bsrc], [BY])
                    for k in range(KC):
                        mm(pG[:, 0:512], sl[:, 16 + 8 * b_ + k, co:co + 128], hT[:, k, tsl], k == 0, k == KC - 1, [Bw, B_hT[tb]], [BG])
                    s_, Bs_ = sgt[it % 2], B_sg[it % 2]
                    it += 1
                    act(s_, pG[:, 0:512], AF.Sigmoid, [BG], [Bs_])
                    if b_ == 0:
                        tt("dve", mtmp[0], pY[:, 0:512], s_, ALU.mult, [BY, Bs_], [B_mt[0]])
                    elif b_ == 1:
                        tt("dve", mtmp[1], pY[:, 0:512], s_, ALU.mult, [BY, Bs_], [B_mt[1]])
                        tt("pool", mtmp[0], mtmp[0], mtmp[1], ALU.add, [B_mt[0], B_mt[1]], [B_mt[0]])
                    else:
                        tt("dve", mtmp[1], pY[:, 0:512], s_, ALU.mult, [BY, Bs_], [B_mt[1]])
                        tt("pool", mT[:, c, tsl], mtmp[0], mtmp[1], ALU.add, [B_mt[0], B_mt[1]], [B_mT[tb]])
        dump("mT", mT, B_mT, [128, KC, S])
        if stop_after == "merge":
            return _finish(nc, SP, dumps)
        assert not PE.pending
        pe_done = Tok(PE.sem, PE.count)
        pool_done = Tok(POOL.sem, POOL.count)
        BIG.release(BIG.lo)
        x2 = BIG.alloc([NT, D], F32)
        B_x2 = [Buf(f"x2_{i}") for i in range(NT)]
        WORK.release(m0)
        xs7f = wg6[1][:].rearrange("p a b -> p (a b)").bitcast(F32)
        xs7 = [xs7f[:, 0:D], xs7f[:, D:2 * D]]
        B_xs7 = [Buf("xs7a"), Buf("xs7b")]
        SP.wait(pe_done)
        DVE.wait(pe_done)
        DVE.wait(pool_done)
        for i in range(NT):
            xt, Bx = xs7[i % 2], B_xs7[i % 2]
            qsp.dma(xt, x_d[i * 128:(i + 1) * 128, :], writes=[Bx])
            for half in range(2):
                pb, Bp = bank()
                for k in range(KC):
                    mm(pb[:, 0:512], mT[:, k, i * 128:(i + 1) * 128], wout[:, k, half * 512:(half + 1) * 512], k == 0, k == KC - 1,
                       [B_mT[i // 4], B_wout], [Bp])
                tt("dve", x2[:, i, half * 512:(half + 1) * 512], pb[:, 0:512], xt[:, half * 512:(half + 1) * 512], ALU.add, [Bp, Bx], [B_x2[i]])
        dump("x2", x2, B_x2, [128, NT, D])
        if stop_after == "x2":
            return _finish(nc, SP, dumps)
        barrier()
        WAR.release(mW6)

        tT = hT
        B_tT = [Buf(f"tT{i}") for i in range(4)]
        wsl8 = [WAR.alloc([12288], BF16) for _ in range(2)]
        B_w8 = [Buf("w8a"), Buf("w8b")]
        W8 = Arena(arena_t, WORK.lo, WORK.hi)
        wr = W8.alloc([KC, 20], F32)
        rb = W8.alloc([20], F32)
        lgl = W8.alloc([NT, 20], F32)
        comb = W8.alloc([NT, 16], F32)
        off_t = W8.top
        tst = [W8.alloc([KC, 128], F32) for _ in range(1)]
        W8b = Arena(arena_t, off_t, off_t + 4 * KB)
        thi = W8b.alloc([D], BF16)
        tlo = W8b.alloc([D], BF16)
        tlT = W8.alloc([KC, 128], BF16)
        whi = W8.alloc([KC, 20], BF16)
        wlo = W8.alloc([KC, 20], BF16)
        tb_ = [W8.alloc([D], F32) for _ in range(1)]
        junk8 = W8.alloc([D], BF16)
        rt = {n: W8.alloc([NT, 4], F32) for n in ["ohg", "eg", "ig", "eq", "ee", "w"]}
        rt4 = W8.alloc([NT, 4, 4], F32)
        r1 = {n: W8.alloc([NT], F32) for n in ["gmax", "gsum", "m1", "m2", "se"]}
        hid = [W8.alloc([4, 512], BF16) for _ in range(2)]
        sa2 = W8.alloc([1024], BF16)
        sa = [sa2[:, 0:512], sa2[:, 512:1024]]
        B8 = {n: Buf(n) for n in ["wr", "rb", "lgl", "comb", "tst", "tb", "junk", "rt", "hid0", "hid1", "sa0", "sa1", "thi", "tlo", "tlT"]}
        w_eg_v = w_eg_d.rearrange("e (k p) f -> e p k f", p=128)
        w_eu_v = w_eu_d.rearrange("e (k p) f -> e p k f", p=128)
        w_ed_v = w_ed_d.rearrange("e (k p) f -> e p k f", p=128)

        def load_w8(e):
            sl, Bw = wsl8[e % 2], B_w8[e % 2]
            qpl.dma(sl[:, 0:4096].rearrange("p (k f) -> p k f", k=8), w_eg_v[e], writes=[Bw])
            qpl.dma(sl[:, 4096:8192].rearrange("p (k f) -> p k f", k=8), w_eu_v[e], writes=[Bw])
            qpl.dma(sl[:, 8192:12288].rearrange("p (k f) -> p k f", k=4), w_ed_v[e], writes=[Bw])

        if not lite:
            load_w8(0)
        qsp.dma(gA, g_ffn_d.partition_broadcast(128), writes=[B_gA])
        import os
        if os.environ.get("SKIPWR") != "1":
            qsp.dma(wr[:, :, 0:4], w_rg_d.rearrange("(k p) f -> p k f", p=128), writes=[B8["wr"]])
            qsp.dma(wr[:, :, 4:20], w_re_d.rearrange("(k p) f -> p k f", p=128), writes=[B8["wr"]])
            qsp.dma(rb[:, 0:4], b_rg_d.partition_broadcast(128), writes=[B8["rb"]])
            qsp.dma(rb[:, 4:20], b_re_d.partition_broadcast(128), writes=[B8["rb"]])
        cp("dve", whi, wr, [B8["wr"]], [B8["wr"]])
        tt("dve", wlo, wr, whi, ALU.subtract, [B8["wr"]], [B8["wr"]])
        hidf = [hid[j][:].rearrange("p a b -> p (a b)") for j in range(2)]
        tbs = [tb_[0], hidf[0].bitcast(F32)]
        B_tbs = [[B8["tb"]], [B8["hid0"]]]
        this = [thi, hidf[1][:, 0:D]]
        tlos = [tlo, hidf[1][:, D:2 * D]]
        B_this = [[B8["thi"]], [B8["hid1"]]]
        B_tlos = [[B8["tlo"]], [B8["hid1"]]]
        tlTs = [tlT, sa2.rearrange("p (k t) -> p k t", k=KC)]
        B_tlTs = [[B8["tlT"]], [B8["sa0"], B8["sa1"]]]

        def route_stage1(i):
            u = i % 2
            tbu, thu, tlu = tbs[u], this[u], tlos[u]
            Btb, Bth, Btl = B_tbs[u], B_this[u], B_tlos[u]
            ss = small[:, 8 + (i % 8):9 + (i % 8)]
            Bs = B_ss[8 + i % 8]
            act(junk8, x2[:, i, :], AF.Square, [B_x2[i]], [B8["junk"], Bs], accum_out=ss)
            act(ss, ss, AF.Sqrt, [Bs], [Bs], scale=1.0 / D, bias=1e-6)
            DVE.op(lambda: nc.vector.reciprocal(out=ss, in_=ss), [Bs], [Bs])
            stt(tbu, x2[:, i, :], ss, gA, ALU.mult, ALU.mult, [B_x2[i], Bs, B_gA], Btb)
            cp("act", thu, tbu, Btb, Bth)
            tt("dve", tlu, tbu, thu, ALU.subtract, Btb + Bth, Btl)

        def route_stage1b(i):
            u = i % 2
            thu, tlu = this[u], tlos[u]
            Bth, Btl = B_this[u], B_tlos[u]
            pb, Bp = bank()
            pT = pb[:].bitcast(BF16)
            for k in range(KC):
                tr(pT[:, k * 128:(k + 1) * 128], thu[:, k * 128:(k + 1) * 128], ident_b, Bth + [B_ident], [Bp], k == KC - 1)
            cp("act", tT[:, :, i * 128:(i + 1) * 128], pT.rearrange("p (k t) -> p k t", k=KC), [Bp], [B_tT[i // 4]])
            pb, Bp = bank()
            pT = pb[:].bitcast(BF16)
            for k in range(KC):
                tr(pT[:, k * 128:(k + 1) * 128], tlu[:, k * 128:(k + 1) * 128], ident_b, Btl + [B_ident], [Bp], k == KC - 1)
            cp("dve", tlTs[u], pT.rearrange("p (k t) -> p k t", k=KC), [Bp], B_tlTs[u])

        def route_stage2(i):
            u = i % 2
            pb, Bp = bank()
            nmm = 0
            for (lt, wv_) in [("hi", whi), ("lo", whi), ("hi", wlo)]:
                for k in range(KC):
                    lhs = tT[:, k, i * 128:(i + 1) * 128] if lt == "hi" else tlTs[u][:, k, :]
                    Rl = ([B_tT[i // 4]] if lt == "hi" else B_tlTs[u]) + [B8["wr"]]
                    mm(pb[:, 0:20], lhs, wv_[:, k, :], nmm == 0, nmm == 3 * KC - 1, Rl, [Bp])
                    nmm += 1
            tt("dve", lgl[:, i, :], pb[:, 0:20], rb, ALU.add, [Bp, B8["rb"]], [B8["lgl"]])

        route_stage1(0)
        for i in range(NT):
            if i + 1 < NT:
                route_stage1(i + 1)
            route_stage1b(i)
            if i >= 1:
                route_stage2(i - 1)
        route_stage2(NT - 1)
        if stop_after == "moe_lg":
            dump("lgl", lgl, [B8["lgl"]], [128, NT, 20])
            return _finish(nc, SP, dumps)
        Lg = lgl[:, :, 0:4]
        Le = lgl[:, :, 4:20].rearrange("p t (g e) -> p t g e", g=4)
        RB = [B8["lgl"], B8["rt"]]
        WB_ = [B8["rt"]]

        def bc4(ap):
            return ap.unsqueeze(2).broadcast_to([128, NT, 4])

        red(r1["gmax"], Lg, ALU.max, RB, WB_)
        tt("dve", rt["ohg"], Lg, bc4(r1["gmax"]), ALU.is_equal, RB, WB_)
        tt("dve", rt["eg"], Lg, bc4(r1["gmax"]), ALU.subtract, RB, WB_)
        act(rt["eg"], rt["eg"], AF.Exp, RB, WB_)
        red(r1["gsum"], rt["eg"], ALU.add, RB, WB_)
        tt("dve", rt4, Le, rt["ohg"].unsqueeze(3).broadcast_to([128, NT, 4, 4]), ALU.mult, RB, WB_)
        red(rt["ig"], rt4.rearrange("p t g e -> p t e g"), ALU.add, RB, WB_)
        red(r1["m1"], rt["ig"], ALU.max, RB, WB_)
        tt("dve", rt["eq"], rt["ig"], bc4(r1["m1"]), ALU.is_equal, RB, WB_)
        stt(rt["eq"], rt["eq"], -1e30, rt["ig"], ALU.mult, ALU.add, RB, WB_)
        red(r1["m2"], rt["eq"], ALU.max, RB, WB_)
        tt("dve", rt["eq"], rt["ig"], bc4(r1["m2"]), ALU.is_ge, RB, WB_)
        tt("dve", rt["ee"], rt["ig"], bc4(r1["m1"]), ALU.subtract, RB, WB_)
        act(rt["ee"], rt["ee"], AF.Exp, RB, WB_)
        tt("dve", rt["ee"], rt["ee"], rt["eq"], ALU.mult, RB, WB_)
        red(r1["se"], rt["ee"], ALU.add, RB, WB_)
        tt("dve", r1["se"], r1["se"], r1["gsum"], ALU.mult, RB, WB_)
        DVE.op(lambda: nc.vector.reciprocal(out=r1["se"], in_=r1["se"]), RB, WB_)
        tt("dve", rt["w"], rt["ee"], bc4(r1["se"]), ALU.mult, RB, WB_)
        tt("dve", comb[:].rearrange("p t (g e) -> p t g e", g=4), rt["ohg"].unsqueeze(3).broadcast_to([128, NT, 4, 4]),
           rt["w"].unsqueeze(2).broadcast_to([128, NT, 4, 4]), ALU.mult, RB, [B8["comb"]])
        dump("comb", comb, [B8["comb"]], [128, NT, 16])
        dump("tT", tT, B_tT, [128, KC, S])
        if stop_after == "route":
            return _finish(nc, SP, dumps)
        assert not PE.pending
        route_pe_tok = Tok(PE.sem, PE.count)
        qsp.dma(gB, g_fin_d.partition_broadcast(128), writes=[B_gB])
        ob = [tst[0].rearrange("p k t -> p (k t)"), tb_[0]]
        B_ob = [B8["tst"], B8["tb"]]
        outs = []

        def final_tile(i):
            ss = small[:, 16 + (i % 8):17 + (i % 8)]
            act(junk8, x2[:, i, :], AF.Square, [B_x2[i]], [B8["junk"], B_small], accum_out=ss)
            act(ss, ss, AF.Sqrt, [B_small], [B_small], scale=1.0 / D, bias=1e-6)
            DVE.op(lambda: nc.vector.reciprocal(out=ss, in_=ss), [B_small], [B_small])
            stt(ob[i % 2], x2[:, i, :], ss, gB, ALU.mult, ALU.mult, [B_x2[i], B_small, B_gB], [B_ob[i % 2]])
            outs.append(qsp.dma(out_d[i * 128:(i + 1) * 128, :], ob[i % 2], reads=[B_ob[i % 2]]))

        itc = [0]

        def wviews(e):
            sl, Bw = wsl8[e % 2], B_w8[e % 2]
            wgv = sl[:, 0:4096].rearrange("p (k f) -> p k f", k=8)
            wuv = sl[:, 4096:8192].rearrange("p (k f) -> p k f", k=8)
            wdv = sl[:, 8192:12288].rearrange("p (k f) -> p k f", k=4)
            return wgv, wuv, wdv, Bw

        def gate_up(e, tb):
            wgv, wuv, wdv, Bw = wviews(e)
            tsl = slice(tb * 512, (tb + 1) * 512)
            hd, Bhd = hid[tb % 2], B8[f"hid{tb % 2}"]
            for fc in range(4):
                pA, BA = bank()
                pU, BU = bank()
                for k in range(KC):
                    mm(pA[:, 0:512], wgv[:, k, fc * 128:(fc + 1) * 128], tT[:, k, tsl], k == 0, k == KC - 1, [Bw, B_tT[tb]], [BA])
                for k in range(KC):
                    mm(pU[:, 0:512], wuv[:, k, fc * 128:(fc + 1) * 128], tT[:, k, tsl], k == 0, k == KC - 1, [Bw, B_tT[tb]], [BU])
                s_, Bs_ = sa[itc[0] % 2], B8[f"sa{itc[0] % 2}"]
                itc[0] += 1
                act(s_, pA[:, 0:512], AF.Silu, [BA], [Bs_])
                tt("dve", hd[:, fc, :], pU[:, 0:512], s_, ALU.mult, [BU, Bs_], [Bhd])

        def down(e, tb):
            wgv, wuv, wdv, Bw = wviews(e)
            hd, Bhd = hid[tb % 2], B8[f"hid{tb % 2}"]
            for tl in range(4):
                i = tb * 4 + tl
                for half in range(2):
                    pb, Bp = bank()
                    for fc in range(4):
                        mm(pb[:, 0:512], hd[:, fc, tl * 128:(tl + 1) * 128], wdv[:, fc, half * 512:(half + 1) * 512], fc == 0, fc == 3,
                           [Bhd, Bw], [Bp])
                    xs_ = x2[:, i, half * 512:(half + 1) * 512]
                    stt(xs_, pb[:, 0:512], comb[:, i, e:e + 1], xs_, ALU.mult, ALU.add, [Bp, B8["comb"], B_x2[i]], [B_x2[i]])
                if e == 15:
                    if i == 0:
                        DVE.wait(route_pe_tok)
                    final_tile(i)

        steps = [(e, tb) for e in range(16) for tb in range(4)]
        load_w8(1)
        gate_up(*steps[0])
        for si, (e, tb) in enumerate(steps):
            if si + 1 < len(steps):
                gate_up(*steps[si + 1])
            down(e, tb)
            if tb == 3 and e + 2 < 16:
                load_w8(e + 2)
        dump("x3", x2, B_x2, [128, NT, D])
        for t_ in outs + dumps:
            SP.wait(t_)
        stats_ = {e.name: (e.nins, e.count) for e in [PE, ACT, DVE, POOL, SP]}
        build.stats = stats_
    return nc


def _finish(nc, SP, dumps):
    for t_ in dumps:
        SP.wait(t_)
    return nc


_CONSTS = None


def make_in_maps(inputs, lite=False):
    global _CONSTS
    if _CONSTS is None:
        _CONSTS = _host_constants()
    f = lambda a: np.ascontiguousarray(np.asarray(a, dtype=np.float32))
    shared = {
        "g_mix": f(inputs["g_mix"]).reshape(D),
        "w_in": f(inputs["w_in"]).reshape(D, 8192),
        "ret_decay_fwd": f(inputs["ret_decay_fwd"]).reshape(4),
        "ret_decay_bwd": f(inputs["ret_decay_bwd"]).reshape(4),
        "ret_norm_gain": f(inputs["ret_norm_gain"]).reshape(D),
        "w_ret_o": f(inputs["w_ret_o"]).reshape(D, D),
        "rpbT": _rpb_layout(f(inputs["na_rpb"]).reshape(8, 15, 31)),
        "w_na_o": f(inputs["w_na_o"]).reshape(512, D),
        "g_mem": f(inputs["g_mem"]).reshape(D),
        "w_mem_kv": f(inputs["w_mem_kv"]).reshape(D, D),
        "w_xa_o": f(inputs["w_xa_o"]).reshape(512, D),
        "w_out": f(inputs["w_out"]).reshape(D, D),
        "g_ffn": f(inputs["g_ffn"]).reshape(D),
        "w_router_group": f(inputs["w_router_group"]).reshape(D, 4),
        "b_router_group": f(inputs["b_router_group"]).reshape(4),
        "w_router_expert": f(inputs["w_router_expert"]).reshape(D, 16),
        "b_router_expert": f(inputs["b_router_expert"]).reshape(16),
        "w_exp_gate": f(inputs["w_exp_gate"]).reshape(16, D, 512),
        "w_exp_up": f(inputs["w_exp_up"]).reshape(16, D, 512),
        "w_exp_down": f(inputs["w_exp_down"]).reshape(16, 512, D),
        "g_final": f(inputs["g_final"]).reshape(D),
    }
    if lite:
        shared["w_exp_gate"] = np.zeros((1, 128, 512), np.float32)
        shared["w_exp_up"] = np.zeros((1, 128, 512), np.float32)
        shared["w_exp_down"] = np.zeros((1, 128, D), np.float32)
    w_in = shared["w_in"]
    wi = w_in.reshape(KC, 128, 8192)
    def blk(w, lo, hi):
        return w[:, :, lo:hi].transpose(1, 0, 2)
    w6 = np.empty((4, 128, 40, 256), np.float32)
    wro = shared["w_ret_o"].reshape(8, 128, D)
    wna = shared["w_na_o"].reshape(4, 128, D)
    wxa = shared["w_xa_o"].reshape(4, 128, D)
    for cp_ in range(4):
        lo, hi = cp_ * 256, (cp_ + 1) * 256
        w6[cp_, :, 0:8] = blk(wro, lo, hi)
        w6[cp_, :, 8:12] = blk(wna, lo, hi)
        w6[cp_, :, 12:16] = blk(wxa, lo, hi)
        for b_ in range(3):
            w6[cp_, :, 16 + 8 * b_:24 + 8 * b_] = blk(wi, 5120 + b_ * 1024 + lo, 5120 + b_ * 1024 + hi)
    shared["w6"] = w6.reshape(4, 128, 40 * 256)
    wret = np.empty((4, 128, KC, 768), np.float32)
    for h in range(4):
        wret[h, :, :, 0:128] = blk(wi, h * 128, (h + 1) * 128)
        wret[h, :, :, 128:256] = blk(wi, 512 + h * 128, 512 + (h + 1) * 128)
        wret[h, :, :, 256:512] = blk(wi, 1024 + h * 256, 1024 + (h + 1) * 256)
        wret[h, :, :, 512:768] = blk(wi, 2048 + h * 256, 2048 + (h + 1) * 256)
    shared["wret"] = wret.reshape(4, 128, KC * 768)
    shared["gn_pk"] = np.ascontiguousarray(shared["ret_norm_gain"].reshape(KC, 128).T)
    shared.update(_CONSTS)
    x = f(inputs["x"])
    mem = f(inputs["mem"])
    maps = []
    for b in range(x.shape[0]):
        m = dict(shared)
        m["x"] = x[b]
        m["mem"] = mem[b]
        maps.append(m)
    return maps


def kernel(**inputs):
    nc = build()
    in_maps = make_in_maps(inputs)
    res = run_bass_kernel_spmd(nc, in_maps, core_ids=list(range(len(in_maps))))
    out = np.stack([np.asarray(r["out"], dtype=np.float32) for r in res.results], axis=0)
    return out
```

```python
import numpy as np
from contextlib import ExitStack
import concourse.bass as bass
import concourse.mybir as mybir
from concourse.bass_utils import run_bass_kernel_spmd

F32 = mybir.dt.float32
BF16 = mybir.dt.bfloat16
AF = mybir.ActivationFunctionType
ALU = mybir.AluOpType
AX = mybir.AxisListType

D = 1024
S = 2048
NT = 16
KC = 8
MEM = 256
NEGBIG = -240000.0
QSCALE = 128.0 ** -0.5


class Tok:
    __slots__ = ("sem", "val")

    def __init__(self, sem=None, val=None):
        self.sem = sem
        self.val = val


class Buf:
    __slots__ = ("name", "w", "r")

    def __init__(self, name=""):
        self.name = name
        self.w = None
        self.r = []

    def add_read(self, tok):
        self.r.append(tok)
        if len(self.r) > 24:
            best = {}
            keep = []
            for t in self.r:
                if t.val is None:
                    keep.append(t)
                else:
                    k = id(t.sem)
                    if k not in best or best[k].val < t.val:
                        best[k] = t
            self.r = keep + list(best.values())


class Eng:
    def __init__(self, name, h, sem):
        self.name = name
        self.h = h
        self.sem = sem
        self.count = 0
        self.pending = []
        self.waited = {}
        self.nins = 0

    def wait(self, tok):
        if tok is None:
            return
        assert tok.val is not None, f"unresolved token waited by {self.name}"
        key = id(tok.sem)
        if self.waited.get(key, 0) >= tok.val:
            return
        self.h.wait_ge(tok.sem, tok.val)
        self.waited[key] = tok.val

    def _deps(self, reads, writes):
        deps = []
        for b in reads:
            if b.w is not None:
                deps.append(b.w)
        for b in writes:
            deps.extend(b.r)
            if b.w is not None:
                deps.append(b.w)
        return deps

    def op(self, fn, reads=(), writes=(), sig=True):
        for d in self._deps(reads, writes):
            if d.val is None:
                assert any(d is p for p in self.pending), f"unresolved dep on {self.name}"
                continue
            self.wait(d)
        ins = fn()
        self.nins += 1
        tok = Tok(self.sem, None)
        self.pending.append(tok)
        if sig:
            self.count += 1
            ins.then_inc(self.sem, 1)
            for t in self.pending:
                t.val = self.count
            self.pending = []
        for b in reads:
            b.add_read(tok)
        for b in writes:
            b.w = tok
            b.r = []
        return tok


class DmaQ:
    def __init__(self, eng, sems):
        self.eng = eng
        self.sems = sems
        self.cnt = [0] * len(sems)
        self.i = 0

    def dma(self, out, in_, reads=(), writes=(), **kw):
        e = self.eng
        for d in e._deps(reads, writes):
            e.wait(d)
        k = self.i % len(self.sems)
        self.i += 1
        sem = self.sems[k]
        if self.cnt[k] > 0:
            e.wait(Tok(sem, self.cnt[k]))
        self.cnt[k] += 16
        e.h.dma_start(out=out, in_=in_, **kw).then_inc(sem, 16)
        tok = Tok(sem, self.cnt[k])
        for b in reads:
            b.add_read(tok)
        for b in writes:
            b.w = tok
            b.r = []
        return tok


def _dt_size(dt):
    return 2 if dt == BF16 else 4


class Arena:
    def __init__(self, tensor, lo, hi):
        self.t = tensor
        self.lo = lo
        self.hi = hi
        self.top = lo

    def alloc(self, shape, dt):
        n = 1
        for s in shape:
            n *= s
        nb = n * _dt_size(dt)
        nb = (nb + 63) // 64 * 64
        off = self.top
        assert off + nb <= self.hi, f"arena overflow: need {nb} at {off - self.lo} of {self.hi - self.lo}"
        self.top = off + nb
        ap = self.t[:, off // 4:(off + nb) // 4]
        if dt != F32:
            ap = ap.bitcast(dt)
        ap = ap[:, 0:n]
        if len(shape) == 1:
            return ap
        names = [f"d{i}" for i in range(len(shape))]
        pat = "p (" + " ".join(names) + ") -> p " + " ".join(names)
        kw = {names[i]: shape[i] for i in range(1, len(shape))}
        return ap.rearrange(pat, **kw)

    def mark(self):
        return self.top

    def release(self, m):
        self.top = m


def _host_constants():
    c = {}
    c["c_ident"] = np.eye(128, dtype=np.float32)
    half = 64
    inv_freq = (10000.0 ** (-np.arange(half, dtype=np.float32) / half)).astype(np.float32)
    pos = np.arange(S, dtype=np.float32)
    ang = (pos[:, None] * inv_freq[None, :]).astype(np.float32)
    rope = np.stack([np.cos(ang), np.sin(ang)], axis=1).astype(np.float32)
    c["c_rope"] = np.ascontiguousarray(rope.reshape(NT, 128, 2, 64).transpose(1, 0, 2, 3))
    j = np.arange(128, dtype=np.float32)[:, None]
    i = np.arange(128, dtype=np.float32)[None, :]
    ret = np.zeros((128, 6, 128), np.float32)
    ret[:, 0, :] = -np.maximum(i - j, 0)
    ret[:, 1, :] = (i >= j)
    ret[:, 2, :] = -np.maximum(j - i, 0)
    ret[:, 3, :] = (i < j)
    ret[:, 4, :] = -(i + 1)
    ret[:, 5, :] = -(128 - i)
    c["c_ret"] = ret
    pj = np.zeros((128, 2), np.float32)
    pj[:, 0] = -(127 - np.arange(128))
    pj[:, 1] = -np.arange(128)
    c["c_pj"] = pj
    qc = np.arange(64)[None, :]
    kc = np.arange(64)[:, None]
    cs = np.clip(qc - 8, 0, 48)
    ok = ((kc >= cs) & (kc < cs + 16)).astype(np.float32)
    na = np.zeros((128, 2, 64), np.float32)
    na[:, 0, :] = np.concatenate([ok, ok], 0) * 8.0
    na[:, 1, :] = (np.concatenate([ok, ok], 0) - 1.0) * (-NEGBIG)
    c["c_na"] = na
    return c


def _rpb_layout(rpb):
    kc = np.arange(64)[:, None]
    qc = np.arange(64)[None, :]
    d = kc - qc + 15
    valid = (d >= 0) & (d <= 30)
    dcl = np.clip(d, 0, 30)
    g = rpb[:, :, dcl]
    g = np.where(valid[None, None], g, np.float32(0)).astype(np.float32)
    g = g[:, ::-1]
    t = np.ascontiguousarray(g.transpose(2, 0, 1, 3))
    return np.ascontiguousarray(np.concatenate([t, t], 0))


def _na_plan():
    def row_start(r):
        return min(max(r - 4, 0), 24)
    plan = []
    cfgs = {}
    for t in range(16):
        lst = []
        for kt in range(16):
            blocks = []
            anyv = False
            for a in range(2):
                for b in range(2):
                    rq = 2 * t + b
                    rk = 2 * kt + a
                    v = row_start(rq) <= rk < row_start(rq) + 8
                    dr = rk - rq + 7
                    blocks.append((14 - dr) if v else None)
                    anyv = anyv or v
            if anyv:
                key = tuple(blocks)
                if key not in cfgs:
                    cfgs[key] = len(cfgs)
                lst.append((kt, cfgs[key]))
        plan.append(lst)
    return plan, cfgs


def build(dbg=(), stop_after=None, lite=False):
    nc = bass.Bass("TRN2", target_bir_lowering=False)
    dbg = set(dbg)

    def din(name, shape):
        return nc.dram_tensor(name, list(shape), F32, kind="ExternalInput").ap()

    x_d = din("x", [S, D])
    mem_d = din("mem", [MEM, D])
    g_mix_d = din("g_mix", [D])
    w_in_d = din("w_in", [D, 8192])
    decf_d = din("ret_decay_fwd", [4])
    decb_d = din("ret_decay_bwd", [4])
    gn_d = din("ret_norm_gain", [D])
    w_ro_d = din("w_ret_o", [D, D])
    rpbT_d = din("rpbT", [128, 8, 15, 64])
    w_nao_d = din("w_na_o", [512, D])
    g_mem_d = din("g_mem", [D])
    w_kv_d = din("w_mem_kv", [D, D])
    w_xao_d = din("w_xa_o", [512, D])
    w_out_d = din("w_out", [D, D])
    g_ffn_d = din("g_ffn", [D])
    w_rg_d = din("w_router_group", [D, 4])
    b_rg_d = din("b_router_group", [4])
    w_re_d = din("w_router_expert", [D, 16])
    b_re_d = din("b_router_expert", [16])
    if lite:
        w_eg_d = din("w_exp_gate", [1, 128, 512])
        w_eu_d = din("w_exp_up", [1, 128, 512])
        w_ed_d = din("w_exp_down", [1, 128, D])
    else:
        w_eg_d = din("w_exp_gate", [16, D, 512])
        w_eu_d = din("w_exp_up", [16, D, 512])
        w_ed_d = din("w_exp_down", [16, 512, D])
    g_fin_d = din("g_final", [D])
    w6_d = din("w6", [4, 128, 40 * 256])
    wret_d = din("wret", [4, 128, KC * 768])
    gnpk_d = din("gn_pk", [128, KC])
    c_ident_d = din("c_ident", [128, 128])
    c_rope_d = din("c_rope", [128, NT, 2, 64])
    c_ret_d = din("c_ret", [128, 6, 128])
    c_pj_d = din("c_pj", [128, 2])
    c_na_d = din("c_na", [128, 2, 64])
    out_d = nc.dram_tensor("out", [S, D], F32, kind="ExternalOutput").ap()

    na_plan, na_cfgs = _na_plan()
    NCFG = len(na_cfgs)

    es = ExitStack()
    with es:
        KB = 1024
        TOTAL = 196 * KB
        arena_t = es.enter_context(nc.sbuf_tensor("arena", [128, TOTAL // 4], F32))
        CONST = Arena(arena_t, 0, 20 * KB)
        HTR = Arena(arena_t, 20 * KB, 52 * KB)
        BIG = Arena(arena_t, 52 * KB, 116 * KB)
        WAR = Arena(arena_t, 116 * KB, 164 * KB)
        WORK = Arena(arena_t, 164 * KB, 196 * KB)

        pbanks = [es.enter_context(nc.psum_tensor(f"pb{i}", [128, 512], F32)) for i in range(8)]
        PB = [Buf(f"pb{i}") for i in range(8)]
        bank_i = [0]

        def bank():
            k = bank_i[0] % 8
            bank_i[0] += 1
            return pbanks[k], PB[k]

        def sem(name):
            return es.enter_context(nc.semaphore(name))

        PE = Eng("pe", nc.tensor, sem("s_pe"))
        ACT = Eng("act", nc.scalar, sem("s_act"))
        DVE = Eng("dve", nc.vector, sem("s_dve"))
        POOL = Eng("pool", nc.gpsimd, sem("s_pool"))
        SP = Eng("sp", nc.sync, sem("s_sp"))
        qsp = DmaQ(SP, [sem(f"dsp{i}") for i in range(16)])
        qpl = DmaQ(POOL, [sem(f"dpl{i}") for i in range(12)])
        es.enter_context(nc.Block())
        ENG = {"act": ACT, "dve": DVE, "pool": POOL}
        HND = {"act": nc.scalar, "dve": nc.vector, "pool": nc.gpsimd}

        def barrier():
            assert not PE.pending
            engs = [PE, ACT, DVE, POOL]
            for e in engs + [SP]:
                for o in engs:
                    if o is not e and o.count > 0:
                        e.wait(Tok(o.sem, o.count))

        def mm(out, lhsT, rhs, start, stop, R, W, sig=None):
            if sig is None:
                sig = stop
            return PE.op(lambda: nc.tensor.matmul(out, lhsT=lhsT, rhs=rhs, start=start, stop=stop), R, W, sig)

        def tr(out, in_, ident, R, W, sig):
            return PE.op(lambda: nc.tensor.transpose(out=out, in_=in_, identity=ident), R, W, sig)

        def act(out, in_, func, R, W, **kw):
            return ACT.op(lambda: nc.scalar.activation(out=out, in_=in_, func=func, **kw), R, W)

        def tt(e, out, in0, in1, op, R, W):
            return ENG[e].op(lambda: HND[e].tensor_tensor(out=out, in0=in0, in1=in1, op=op), R, W)

        def ts(e, out, in0, s1, s2, op0, op1, R, W):
            if op1 is None:
                return ENG[e].op(lambda: HND[e].tensor_scalar(out=out, in0=in0, scalar1=s1, scalar2=None, op0=op0), R, W)
            return ENG[e].op(lambda: HND[e].tensor_scalar(out=out, in0=in0, scalar1=s1, scalar2=s2, op0=op0, op1=op1), R, W)

        def stt(out, in0, scalar, in1, op0, op1, R, W):
            return DVE.op(lambda: nc.vector.scalar_tensor_tensor(out=out, in0=in0, scalar=scalar, in1=in1, op0=op0, op1=op1), R, W)

        def cp(e, out, in_, R, W):
            if e == "act":
                return ACT.op(lambda: nc.scalar.copy(out=out, in_=in_), R, W)
            return ENG[e].op(lambda: HND[e].tensor_copy(out=out, in_=in_), R, W)

        def red(out, in_, op, R, W):
            return DVE.op(lambda: nc.vector.tensor_reduce(out=out, in_=in_, axis=AX.X, op=op), R, W)

        dumps = []

        def dump(name, ap, R, shape):
            if name not in dbg:
                return
            dt = nc.dram_tensor("dbg_" + name, list(shape), ap.dtype, kind="ExternalOutput").ap()
            dumps.append(qsp.dma(dt, ap, reads=R))

        ident_f = CONST.alloc([128], F32)
        ident_b = CONST.alloc([128], BF16)
        ones_b = CONST.alloc([128], BF16)
        neg_b = CONST.alloc([128], BF16)
        gA = CONST.alloc([D], F32)
        gB = CONST.alloc([D], F32)
        WT = CONST.alloc([4, 128], F32)
        gfq = CONST.alloc([4, 128], F32)
        gbq = CONST.alloc([4, 128], F32)
        kdec = CONST.alloc([4, 2], F32)
        lg = CONST.alloc([2, 4], F32)
        agam = CONST.alloc([2, 4], F32)
        pj = CONST.alloc([2], F32)
        small = CONST.alloc([64], F32)
        B_ident = Buf("ident")
        B_gA = Buf("gA")
        B_gB = Buf("gB")
        B_ret = Buf("retc")
        B_small = Buf("small")
        B_ss = [Buf(f"ss{i}") for i in range(24)]

        qsp.dma(ident_f, c_ident_d[:, :], writes=[B_ident])
        cp("dve", ident_b, ident_f, [B_ident], [B_ident])
        POOL.op(lambda: nc.gpsimd.memset(ones_b, 1.0), [], [B_ident])
        POOL.op(lambda: nc.gpsimd.memset(neg_b, NEGBIG), [], [B_ident])
        qsp.dma(gA, g_mix_d.partition_broadcast(128), writes=[B_gA])
        m0 = WORK.mark()

        cret = CONST.alloc([6, 128], F32)
        B_cret = Buf("cret")
        tmpb = gB.rearrange("p (a b) -> p a b", a=8)[:, 0:4, :]

        def emit_ret_consts_act():
            qsp.dma(pj, c_pj_d[:, :], writes=[B_ret])
            qsp.dma(lg[:, 0, :], decf_d.partition_broadcast(128), writes=[B_ret])
            qsp.dma(lg[:, 1, :], decb_d.partition_broadcast(128), writes=[B_ret])
            qsp.dma(cret, c_ret_d[:, :, :], writes=[B_cret])
            lgf = lg[:].rearrange("p a b -> p (a b)")
            act(lgf, lgf, AF.Exp, [B_ret], [B_ret], scale=-1.0)
            act(lgf, lgf, AF.Ln, [B_ret], [B_ret], bias=1.0)
            act(agam[:].rearrange("p a b -> p (a b)"), lgf, AF.Exp, [B_ret], [B_ret], scale=-128.0)
            for h in range(4):
                lf = lg[:, 0, h:h + 1]
                lb = lg[:, 1, h:h + 1]
                act(WT[:, h, :], cret[:, 0, :], AF.Exp, [B_cret, B_ret], [B_ret], scale=lf)
                act(tmpb[:, h, :], cret[:, 2, :], AF.Exp, [B_cret, B_ret], [B_gB], scale=lb)
                act(gfq[:, h, :], cret[:, 4, :], AF.Exp, [B_cret, B_ret], [B_ret], scale=lf)
                act(gbq[:, h, :], cret[:, 5, :], AF.Exp, [B_cret, B_ret], [B_ret], scale=lb)
                act(kdec[:, h, 0:1], pj[:, 0:1], AF.Exp, [B_ret], [B_ret], scale=lf)
                act(kdec[:, h, 1:2], pj[:, 1:2], AF.Exp, [B_ret], [B_ret], scale=lb)

        def emit_ret_consts_dve():
            for h in range(4):
                stt(WT[:, h, :], WT[:, h, :], QSCALE, cret[:, 1, :], ALU.mult, ALU.mult, [B_ret, B_cret], [B_ret])
                stt(tmpb[:, h, :], tmpb[:, h, :], QSCALE, cret[:, 3, :], ALU.mult, ALU.mult, [B_gB, B_cret], [B_gB])
                tt("dve", WT[:, h, :], WT[:, h, :], tmpb[:, h, :], ALU.add, [B_ret, B_gB], [B_ret])
            gq = gfq[:].rearrange("p a b -> p (a b)")
            ts("dve", gq, gq, QSCALE, None, ALU.mult, None, [B_ret], [B_ret])
            gq2 = gbq[:].rearrange("p a b -> p (a b)")
            ts("dve", gq2, gq2, QSCALE, None, ALU.mult, None, [B_ret], [B_ret])

        hT = HTR.alloc([KC, S], BF16)
        B_hT = [Buf(f"hT{i}") for i in range(4)]

        def rms_to_fm(src_d, ntiles, gtile, B_g, dstT, B_dst_of, tag):
            m = WORK.mark()
            NX = 3
            xs = [WORK.alloc([D], F32) for _ in range(NX)]
            B_xs = [Buf(f"{tag}xs{i}") for i in range(NX)]
            hb = [WORK.alloc([D], BF16) for _ in range(2)]
            B_hb = [Buf(f"{tag}hb{i}") for i in range(2)]
            junk = WORK.alloc([D], BF16)
            B_junk = Buf(tag + "junk")
            for i in range(min(NX - 1, ntiles)):
                qsp.dma(xs[i % NX], src_d[i * 128:(i + 1) * 128, :], writes=[B_xs[i % NX]])
            def stage_a(i):
                xt, Bx = xs[i % NX], B_xs[i % NX]
                if i + NX - 1 < ntiles:
                    j = i + NX - 1
                    qsp.dma(xs[j % NX], src_d[j * 128:(j + 1) * 128, :], writes=[B_xs[j % NX]])
                ss = small[:, (i % 8):(i % 8) + 1]
                Bs = B_ss[i % 8]
                act(junk, xt, AF.Square, [Bx], [B_junk, Bs], accum_out=ss)
                act(ss, ss, AF.Sqrt, [Bs], [Bs], scale=1.0 / D, bias=1e-6)
                DVE.op(lambda: nc.vector.reciprocal(out=ss, in_=ss), [Bs], [Bs])
                stt(hb[i % 2], xt, ss, gtile, ALU.mult, ALU.mult, [Bx, Bs, B_g], [B_hb[i % 2]])

            stage_a(0)
            for i in range(ntiles):
                if i + 1 < ntiles:
                    stage_a(i + 1)
                pb, Bp = bank()
                pT = pb[:].bitcast(BF16)
                for k in range(KC):
                    tr(pT[:, k * 128:(k + 1) * 128], hb[i % 2][:, k * 128:(k + 1) * 128], ident_b, [B_hb[i % 2], B_ident], [Bp], k == KC - 1)
                cp("act" if i % 2 == 0 else "dve", dstT[:, :, i * 128:(i + 1) * 128],
                   pT.rearrange("p (k t) -> p k t", k=KC), [Bp], [B_dst_of(i)])
            barrier()
            WORK.release(m)

        w_in_v = w_in_d.rearrange("(k p) f -> p k f", p=128)
        ret_w = WAR.alloc([KC, 768], BF16)
        B_retw = Buf("retw")

        def ret_load_w(h):
            qpl.dma(ret_w[:].rearrange("p k f -> p (k f)"), wret_d[h], writes=[B_retw], max_dma_last_dim=8192)

        ret_load_w(0)
        rms_to_fm(x_d, NT, gA, B_gA, hT, lambda i: B_hT[i // 4], "x")
        dump("hT", hT, B_hT, [128, KC, S])
        if stop_after == "h":
            return _finish(nc, SP, dumps)

        zT = BIG.alloc([8, S], BF16)
        oT = BIG.alloc([4, S], BF16)
        xaT = BIG.alloc([4, S], BF16)
        B_zT = Buf("zT")
        B_oT = Buf("oT")
        B_xaT = Buf("xaT")

        def retention():
            mW = WAR.lo
            X1 = Arena(arena_t, WAR.top, WAR.hi)
            X2 = Arena(arena_t, BIG.lo + 32 * KB, BIG.hi)
            rope = X2.alloc([NT, 2, 64], F32)
            v_h = X2.alloc([NT, 256], BF16)
            sg = X2.alloc([NT, 256], BF16)
            qkr = X2.alloc([NT, 2, 128], BF16)
            stf = X1.alloc([NT, 256], BF16)
            stb = X1.alloc([NT, 256], BF16)
            qT = X1.alloc([S], BF16)
            kT = X1.alloc([S], BF16)
            qkfb = [X1.alloc([4, 256], F32) for _ in range(2)]
            rt12 = X1.alloc([2, 4, 2, 64], F32)
            scr = WORK.alloc([8 * KB // 4], F32)
            qdf = WORK.alloc([NT, 128], BF16)
            qdb = WORK.alloc([NT, 128], BF16)
            kdf = WORK.alloc([NT, 128], BF16)
            kdb = WORK.alloc([NT, 128], BF16)
            Sst = WORK.alloc([2, 2, 256], F32)
            PTb = [WORK.alloc([128], BF16) for _ in range(4)]
            junkr = WORK.alloc([256], BF16)
            mv_ = WORK.alloc([8, 4], F32)
            gnb = gB
            t1 = rt12[:, 0]
            t2 = rt12[:, 1]
            Yh = scr[:, 0:2048].rearrange("p (t c) -> p t c", t=8)
            z = qkr[:].rearrange("p t a c -> p t (a c)")
            B = {n: Buf(n) for n in ["rope", "v", "sg", "qkr", "stf", "stb", "qT", "kT", "scr", "qdf", "qdb", "kdf", "kdb",
                                      "junkr", "mv", "PT0", "PT1", "PT2", "PT3", "qkf0", "qkf1", "t12",
                                      "S00", "S01", "S10", "S11", "Yh0", "Yh1", "mv0", "mv1"]}
            qsp.dma(rope, c_rope_d[:, :, :, :], writes=[B["rope"]])

            def z_transposes(hz):
                for e2 in range(2):
                    for half in range(2):
                        pb, Bp = bank()
                        pT = pb[:].bitcast(BF16)
                        for ii in range(8):
                            i = half * 8 + ii
                            tr(pT[:, ii * 128:(ii + 1) * 128], z[:, i, e2 * 128:(e2 + 1) * 128], ident_b, [B["qkr"], B_ident], [Bp], ii == 7)
                        cp("act" if half == 0 else "dve", zT[:, 2 * hz + e2, half * 1024:(half + 1) * 1024], pT, [Bp], [B_zT])

            for h in range(4):
                sl, Bw = ret_w, B_retw
                for i in range(NT):
                    pA, BA = bank()
                    pBk, BBk = bank()
                    for k in range(KC):
                        mm(pA[:, 0:512], hT[:, k, i * 128:(i + 1) * 128], sl[:, k, 0:512], k == 0, k == KC - 1, [B_hT[i // 4], Bw], [BA])
                    for k in range(KC):
                        mm(pBk[:, 0:256], hT[:, k, i * 128:(i + 1) * 128], sl[:, k, 512:768], k == 0, k == KC - 1, [B_hT[i // 4], Bw], [BBk])
                    if i == 1 and h > 0:
                        z_transposes(h - 1)
                    if i == 4 and h == 0:
                        emit_ret_consts_act()
                    qkf, Bqkf = qkfb[(i // 4) % 2], B[f"qkf{(i // 4) % 2}"]
                    cp("act", qkf[:, i % 4, :], pA[:, 0:256], [BA], [Bqkf])
                    cp("act", v_h[:, i, :], pA[:, 256:512], [BA], [B["v"]])
                    act(sg[:, i, :], pBk[:, 0:256], AF.Silu, [BBk], [B["sg"]])
                    if i % 4 == 3:
                        i0 = i - 3
                        src = qkf.rearrange("p t (a b c) -> p t a b c", a=2, b=2)
                        a1 = src[:, :, :, 0, :]
                        a2 = src[:, :, :, 1, :]
                        cosb = rope[:, i0:i0 + 4, 0, :].unsqueeze(2).broadcast_to([128, 4, 2, 64])
                        sinb = rope[:, i0:i0 + 4, 1, :].unsqueeze(2).broadcast_to([128, 4, 2, 64])
                        dst = qkr[:, i0:i0 + 4, :, :].rearrange("p t a (b c) -> p t a b c", b=2)
                        R0 = [Bqkf, B["rope"]]
                        tt("dve", t1, a1, cosb, ALU.mult, R0, [B["t12"]])
                        tt("dve", t2, a2, sinb, ALU.mult, R0, [B["t12"]])
                        tt("dve", dst[:, :, :, 0, :], t1, t2, ALU.subtract, [B["t12"]], [B["qkr"]])
                        tt("dve", t1, a1, sinb, ALU.mult, R0, [B["t12"]])
                        tt("dve", t2, a2, cosb, ALU.mult, R0, [B["t12"]])
                        tt("dve", dst[:, :, :, 1, :], t1, t2, ALU.add, [B["t12"]], [B["qkr"]])
                if h + 1 < 4:
                    ret_load_w(h + 1)
                if h == 0:
                    emit_ret_consts_dve()
                if stop_after == "ret_proj":
                    dump("qkr", qkr, [B["qkr"]], [128, NT, 2, 128])
                    return True
                act(kdf[:], qkr[:, :, 1, :], AF.Identity, [B["qkr"], B_ret], [B["kdf"]], scale=kdec[:, h, 0:1])
                act(kdb[:], qkr[:, :, 1, :], AF.Identity, [B["qkr"], B_ret], [B["kdb"]], scale=kdec[:, h, 1:2])

                def qk_transpose_group(g):
                    a, half = g // 2, g % 2
                    dstT, Bd = (qT, B["qT"]) if a == 0 else (kT, B["kT"])
                    pb, Bp = bank()
                    pT = pb[:].bitcast(BF16)
                    for ii in range(8):
                        i = half * 8 + ii
                        tr(pT[:, ii * 128:(ii + 1) * 128], qkr[:, i, a, :], ident_b, [B["qkr"], B_ident], [Bp], ii == 7)
                    cp("act" if half == 0 else "dve", dstT[:, half * 1024:(half + 1) * 1024], pT, [Bp], [Bd])

                DVE.op(lambda: nc.vector.memset(Sst[:, 1, 0, :], 0.0), [], [B["S10"]])
                DVE.op(lambda: nc.vector.memset(Sst[:, 1, 1, :], 0.0), [], [B["S11"]])
                if h == 0:
                    POOL.op(lambda: nc.gpsimd.memset(stf[:, 0, :], 0.0), [], [B["stf"]])
                    POOL.op(lambda: nc.gpsimd.memset(stb[:, NT - 1, :], 0.0), [], [B["stb"]])
                for n in range(NT - 1):
                    nb = NT - 1 - n
                    pk, Bk = bank()
                    mm(pk[:, 0:256], kdf[:, n, :], v_h[:, n, :], True, True, [B["kdf"], B["v"]], [Bk], sig=False)
                    mm(pk[:, 256:512], kdb[:, nb, :], v_h[:, nb, :], True, True, [B["kdb"], B["v"]], [Bk], sig=True)
                    po, pn = (n + 1) % 2, n % 2
                    stt(Sst[:, pn, 0, :], Sst[:, po, 0, :], agam[:, 0, h:h + 1], pk[:, 0:256], ALU.mult, ALU.add,
                        [B[f"S{po}0"], Bk, B_ret], [B[f"S{pn}0"]])
                    stt(Sst[:, pn, 1, :], Sst[:, po, 1, :], agam[:, 1, h:h + 1], pk[:, 256:512], ALU.mult, ALU.add,
                        [B[f"S{po}1"], Bk, B_ret], [B[f"S{pn}1"]])
                    cp("act", stf[:, n + 1, :], Sst[:, pn, 0, :], [B[f"S{pn}0"]], [B["stf"]])
                    cp("act", stb[:, nb - 1, :], Sst[:, pn, 1, :], [B[f"S{pn}1"]], [B["stb"]])
                    if n in (0, 3, 6, 9):
                        qk_transpose_group(n // 3)
                qTv = qT.rearrange("p (n c) -> p n c", n=NT)
                tt("dve", qdf[:], qTv, gfq[:, h, :].unsqueeze(1).broadcast_to([128, NT, 128]), ALU.mult, [B["qT"], B_ret], [B["qdf"]])
                tt("dve", qdb[:], qTv, gbq[:, h, :].unsqueeze(1).broadcast_to([128, NT, 128]), ALU.mult, [B["qT"], B_ret], [B["qdb"]])
                if stop_after == "ret_state":
                    dump("stf", stf, [B["stf"]], [128, NT, 256])
                    return True
                def scores(n):
                    pS, BS = bank()
                    cs = slice(n * 128, (n + 1) * 128)
                    mm(pS[:, 0:128], kT[:, cs], qT[:, cs], True, True, [B["kT"], B["qT"]], [BS])
                    tt("dve", PTb[n % 4], pS[:, 0:128], WT[:, h, :], ALU.mult, [BS, B_ret], [B[f"PT{n % 4}"]])

                def gn_batch(b):
                    hb_ = b % 2
                    n0 = 4 * b
                    Yq = Yh[:, hb_ * 4:(hb_ + 1) * 4, :]
                    Bq, Bmv = B[f"Yh{hb_}"], B[f"mv{hb_}"]
                    mvq = mv_[:, hb_ * 4:(hb_ + 1) * 4, :]
                    msum = mvq[:, :, 0]
                    vsum = mvq[:, :, 1]
                    ts("dve", msum, msum, 1.0 / 256, None, ALU.mult, None, [Bmv], [Bmv])
                    mb_ = msum.unsqueeze(2).broadcast_to([128, 4, 256])
                    tt("dve", Yq, Yq, mb_, ALU.subtract, [Bq, Bmv], [Bq])
                    for j in range(4):
                        act(junkr, Yq[:, j, :], AF.Square, [Bq], [B["junkr"], Bmv], accum_out=mvq[:, j, 1:2])
                    act(vsum, vsum, AF.Sqrt, [Bmv], [Bmv], scale=1.0 / 256, bias=1e-5)
                    DVE.op(lambda: nc.vector.reciprocal(out=vsum, in_=vsum), [Bmv], [Bmv])
                    rb_ = vsum.unsqueeze(2).broadcast_to([128, 4, 256])
                    tt("dve", Yq, Yq, rb_, ALU.mult, [Bq, Bmv], [Bq])
                    tt("dve", z[:, n0:n0 + 4, :], Yq, sg[:, n0:n0 + 4, :], ALU.mult, [Bq, B["sg"]], [B["qkr"]])

                LOOK = 2
                for n in range(LOOK):
                    scores(n)
                for n in range(NT):
                    if n + LOOK < NT:
                        scores(n + LOOK)
                    PT, BPT = PTb[n % 4], B[f"PT{n % 4}"]
                    pY, BY = bank()
                    mm(pY[:, 0:256], PT, v_h[:, n, :], True, False, [BPT, B["v"]], [BY])
                    mm(pY[:, 0:256], qdf[:, n, :], stf[:, n, :], False, False, [B["qdf"], B["stf"]], [BY])
                    mm(pY[:, 0:256], qdb[:, n, :], stb[:, n, :], False, True, [B["qdb"], B["stb"]], [BY])
                    hb_ = (n // 4) % 2
                    Yq = Yh[:, hb_ * 4:(hb_ + 1) * 4, :]
                    mvq = mv_[:, hb_ * 4:(hb_ + 1) * 4, :]
                    act(Yq[:, n % 4, :], pY[:, 0:256], AF.Identity, [BY], [B[f"Yh{hb_}"], B[f"mv{hb_}"]], accum_out=mvq[:, n % 4, 0:1])
                    if n % 4 == 3 and n // 4 >= 1:
                        gn_batch(n // 4 - 1)
                gn_batch(3)
                if h == 3:
                    z_transposes(h)
                if stop_after == "ret_h0":
                    return True
            barrier()
            WAR.release(mW)
            WORK.release(m0)

        if retention():
            return _finish(nc, SP, dumps)
        dump("zT", zT, [B_zT], [128, 8, S])
        if stop_after == "ret":
            return _finish(nc, SP, dumps)

        def natten():
            mW = WAR.mark()
            wv = WAR.alloc([KC, 512], BF16)
            wqk = [WAR.alloc([KC, 256], BF16) for _ in range(2)]
            nva = WAR.alloc([NT, 8, 65], BF16)
            masks = WAR.alloc([2, NCFG, 128], BF16)
            X2 = Arena(arena_t, BIG.lo + 48 * KB, BIG.hi)
            nqT = X2.alloc([S], BF16)
            nkz = X2.alloc([2, S], BF16)
            otm = X2.alloc([NT, 128], BF16)
            Hf = WORK.alloc([2, 15, 64], F32)
            cna = WORK.alloc([2, 64], F32)
            PTn = [WORK.alloc([5, 128], BF16) for _ in range(4)]
            B_mask = [[[Buf(f"mk{hh}_{ci}_{a}") for a in range(2)] for ci in range(NCFG)] for hh in range(2)]
            rc = WORK.alloc([2], F32)
            B = {n: Buf(n) for n in ["wv", "wqk0", "wqk1", "nva", "masks", "nqT", "nkT", "otm", "Hf", "cna", "PT0", "PT1", "PT2", "PT3", "rc"]}
            qpl.dma(wv, w_in_v[:, :, 4096:4608], writes=[B["wv"]])
            qsp.dma(cna, c_na_d[:, :, :], writes=[B["cna"]])
            POOL.op(lambda: nc.gpsimd.memset(nva[:, :, :, 64:65], 1.0), [], [B["nva"]])
            POOL.op(lambda: nc.gpsimd.memset(nkz[64:128, 0, :], 0.0), [], [B["nkT"]])
            POOL.op(lambda: nc.gpsimd.memset(nkz[0:64, 1, :], 0.0), [], [B["nkT"]])

            def load_qk(c):
                sl, Bw = wqk[c % 2], B[f"wqk{c % 2}"]
                qpl.dma(sl[:, :, 0:128], w_in_v[:, :, 3072 + c * 128:3072 + (c + 1) * 128], writes=[Bw])
                qpl.dma(sl[:, :, 128:256], w_in_v[:, :, 3584 + c * 128:3584 + (c + 1) * 128], writes=[Bw])

            load_qk(0)
            for i in range(NT):
                pb, Bp = bank()
                for k in range(KC):
                    mm(pb[:, 0:512], hT[:, k, i * 128:(i + 1) * 128], wv[:, k, :], k == 0, k == KC - 1, [B_hT[i // 4], B["wv"]], [Bp])
                cp("act" if i % 2 == 0 else "dve", nva[:, i, :, 0:64], pb[:, 0:512].rearrange("p (h d) -> p h d", h=8), [Bp], [B["nva"]])
            for c in range(4):
                if c + 1 < 4:
                    load_qk(c + 1)
                sl, Bw = wqk[c % 2], B[f"wqk{c % 2}"]
                for a in range(2):
                    for tb in range(4):
                        pb, Bp = bank()
                        tsl = slice(tb * 512, (tb + 1) * 512)
                        for k in range(KC):
                            mm(pb[:, 0:512], sl[:, k, a * 128:(a + 1) * 128], hT[:, k, tsl], k == 0, k == KC - 1,
                               [Bw, B_hT[tb]], [Bp])
                        if a == 0:
                            cp("act" if tb % 2 == 0 else "dve", nqT[:, tsl], pb[:, 0:512], [Bp], [B["nqT"]])
                        else:
                            cp("act", nkz[0:64, 0, tsl], pb[0:64, 0:512], [Bp], [B["nkT"]])
                            cp("dve", nkz[64:128, 1, tsl], pb[64:128, 0:512], [Bp], [B["nkT"]])
                qsp.dma(Hf, rpbT_d[:, 2 * c:2 * c + 2, :, :], writes=[B["Hf"]])
                Hv = Hf[:].rearrange("p h r q -> p (h r) q")
                tt("dve", Hv, Hv, cna[:, 0, :].unsqueeze(1).broadcast_to([128, 30, 64]), ALU.mult, [B["Hf"], B["cna"]], [B["Hf"]])
                tt("dve", Hv, Hv, cna[:, 1, :].unsqueeze(1).broadcast_to([128, 30, 64]), ALU.add, [B["Hf"], B["cna"]], [B["Hf"]])
                cnt = 0
                for hh in range(2):
                    for key, ci in na_cfgs.items():
                        Bm = B_mask[hh][ci]
                        for a in range(2):
                            ps_ = slice(a * 64, (a + 1) * 64)
                            d0, d1 = key[2 * a], key[2 * a + 1]
                            e = ["dve", "pool", "act"][cnt % 3]
                            cnt += 1
                            if d0 is not None and d1 is not None:
                                assert d1 == d0 + 1
                                cp(e, masks[ps_, hh, ci, :], Hf[ps_, hh, d0:d0 + 2, :].rearrange("p r q -> p (r q)"), [B["Hf"]], [Bm[a]])
                            else:
                                for b_, dd in enumerate((d0, d1)):
                                    fs = slice(b_ * 64, (b_ + 1) * 64)
                                    if dd is None:
                                        cp(e, masks[ps_, hh, ci, fs], neg_b[ps_, 0:64], [B_ident], [Bm[a]])
                                    else:
                                        cp(e, masks[ps_, hh, ci, fs], Hf[ps_, hh, dd, :], [B["Hf"]], [Bm[a]])
                iters = [(t, hh) for t in range(NT) for hh in range(2)]
                pOs = {}

                def stageA(j):
                    t, hh = iters[j]
                    lst = na_plan[t]
                    PT, BPT = PTn[j % 4], B[f"PT{j % 4}"]
                    p1, B1 = bank()
                    p2, B2 = (None, None)
                    if len(lst) > 4:
                        p2, B2 = bank()
                    for idx, (kt, ci) in enumerate(lst):
                        pp, Bpp = (p1, B1) if idx < 4 else (p2, B2)
                        o_ = pp[:, (idx % 4) * 128:(idx % 4 + 1) * 128]
                        mm(o_, nkz[:, hh, kt * 128:(kt + 1) * 128], nqT[:, t * 128:(t + 1) * 128], True, False, [B["nkT"], B["nqT"]], [Bpp], sig=False)
                        last = (idx == min(len(lst), 4) - 1) or (idx == len(lst) - 1)
                        mm(o_, ident_b, masks[:, hh, ci, :], False, True, [B_ident] + B_mask[hh][ci], [Bpp], sig=last)
                    n1 = min(len(lst), 4)
                    act(PT[:, 0:n1, :], p1[:, 0:n1 * 128].rearrange("p (s q) -> p s q", s=n1), AF.Exp, [B1], [BPT], scale=0.125)
                    if len(lst) > 4:
                        act(PT[:, 4, :], p2[:, 0:128], AF.Exp, [B2], [BPT], scale=0.125)

                def stageB(j):
                    t, hh = iters[j]
                    lst = na_plan[t]
                    PT, BPT = PTn[j % 4], B[f"PT{j % 4}"]
                    if hh == 0:
                        pOs[t] = bank()
                    pO, BO = pOs[t]
                    for idx, (kt, ci) in enumerate(lst):
                        mm(pO[:, hh * 65:(hh + 1) * 65], PT[:, idx, :], nva[:, kt, 2 * c + hh, :], idx == 0, idx == len(lst) - 1,
                           [BPT, B["nva"]], [BO])
                    if hh == 1:
                        pOv = pO[:, 0:130].rearrange("p (h d) -> p h d", h=2)
                        DVE.op(lambda: nc.vector.reciprocal(out=rc[:].unsqueeze(2), in_=pOv[:, :, 64:65]), [BO], [B["rc"]])
                        tt("dve", otm[:, t, :].rearrange("p (h d) -> p h d", h=2), pOv[:, :, 0:64],
                           rc[:].unsqueeze(2).broadcast_to([128, 2, 64]), ALU.mult, [BO, B["rc"]], [B["otm"]])

                LOOKN = 3
                for j in range(min(LOOKN, len(iters))):
                    stageA(j)
                for j in range(len(iters)):
                    if j + LOOKN < len(iters):
                        stageA(j + LOOKN)
                    stageB(j)
                for half in range(2):
                    pb, Bp = bank()
                    pT = pb[:].bitcast(BF16)
                    for ii in range(8):
                        i = half * 8 + ii
                        tr(pT[:, ii * 128:(ii + 1) * 128], otm[:, i, :], ident_b, [B["otm"], B_ident], [Bp], ii == 7)
                    cp("act" if half == 0 else "dve", oT[:, c, half * 1024:(half + 1) * 1024], pT, [Bp], [B_oT])
            barrier()
            WAR.release(mW)
            WORK.release(m0)

        natten()
        dump("oT", oT, [B_oT], [128, 4, S])
        if stop_after == "na":
            return _finish(nc, SP, dumps)

        A6 = Arena(arena_t, WAR.lo, WORK.hi)
        mT = A6.alloc([KC, S], BF16)
        wg6_1 = A6.alloc([40, 256], BF16)
        sgt = [A6.alloc([512], F32) for _ in range(2)]
        mtmp = [A6.alloc([512], F32) for _ in range(2)]
        wg6_0 = A6.alloc([40, 256], BF16)
        wg6 = [wg6_0, wg6_1]
        B_w6 = [Buf("w6a"), Buf("w6b")]
        gnpk = CONST.alloc([KC], F32)
        B_gnpk = Buf("gnpk")
        qsp.dma(gnpk, gnpk_d[:, :], writes=[B_gnpk])

        def load_w6(cp_):
            sl, Bw = wg6[cp_ % 2], B_w6[cp_ % 2]
            slf = sl[:].rearrange("p a b -> p (a b)")
            for q_ in range(2):
                qpl.dma(slf[:, q_ * 5120:(q_ + 1) * 5120], w6_d[cp_][:, q_ * 5120:(q_ + 1) * 5120], writes=[Bw], max_dma_last_dim=8192)

        def scale_w6(cp_):
            sl, Bw = wg6[cp_ % 2], B_w6[cp_ % 2]
            for k in range(KC):
                act(sl[:, k, :], sl[:, k, :], AF.Identity, [Bw, B_gnpk], [Bw], scale=gnpk[:, k:k + 1])

        def xattn():
            mW = WAR.mark()
            wkv = WAR.alloc([KC, D], BF16)
            wxq = WAR.alloc([KC, 512], BF16)
            xqT = [WAR.alloc([S], BF16) for _ in range(2)]
            memT = WAR.alloc([KC, MEM], BF16)
            mkT = WAR.alloc([4, MEM], BF16)
            mvv = WAR.alloc([2, 512], BF16)
            B = {n: Buf(n) for n in ["wkv", "wxq", "xq0", "xq1", "memT", "mkT", "mv", "PT0", "PT1", "rc0", "rc1"]}
            qpl.dma(wkv[:, :, 0:512], w_kv_d.rearrange("(k p) f -> p k f", p=128)[:, :, 0:512], writes=[B["wkv"]])
            qpl.dma(wkv[:, :, 512:1024], w_kv_d.rearrange("(k p) f -> p k f", p=128)[:, :, 512:1024], writes=[B["wkv"]])
            qpl.dma(wxq, w_in_v[:, :, 4608:5120], writes=[B["wxq"]])
            qsp.dma(gA, g_mem_d.partition_broadcast(128), writes=[B_gA])
            rms_to_fm(mem_d, 2, gA, B_gA, memT, lambda i: B["memT"], "m")
            load_w6(0)
            PTx = [WORK.alloc([2, 512], BF16) for _ in range(2)]
            rcx = [WORK.alloc([512], F32) for _ in range(2)]
            for h in range(4):
                pb, Bp = bank()
                for k in range(KC):
                    mm(pb[:, 0:256], wkv[:, k, h * 128:(h + 1) * 128], memT[:, k, :], k == 0, k == KC - 1, [B["wkv"], B["memT"]], [Bp])
                cp("act", mkT[:, h, :], pb[:, 0:256], [Bp], [B["mkT"]])
            for mt in range(2):
                pb, Bp = bank()
                for k in range(KC):
                    mm(pb[:, 0:512], memT[:, k, mt * 128:(mt + 1) * 128], wkv[:, k, 512:1024], k == 0, k == KC - 1, [B["wkv"], B["memT"]], [Bp])
                cp("dve", mvv[:, mt, :], pb[:, 0:512], [Bp], [B["mv"]])
            stepsx = [(h, tb) for h in range(4) for tb in range(4)]

            def xq_proj(h):
                xq, Bxq = xqT[h % 2], B[f"xq{h % 2}"]
                for tb in range(4):
                    pb, Bp = bank()
                    for k in range(KC):
                        mm(pb[:, 0:512], wxq[:, k, h * 128:(h + 1) * 128], hT[:, k, tb * 512:(tb + 1) * 512], k == 0, k == KC - 1,
                           [B["wxq"], B_hT[tb]], [Bp])
                    cp("act", xq[:, tb * 512:(tb + 1) * 512], pb[:, 0:512], [Bp], [Bxq])

            def xa_scores(j):
                h, tb = stepsx[j]
                if tb == 0:
                    xq_proj(h)
                xq, Bxq = xqT[h % 2], B[f"xq{h % 2}"]
                PT, BPT = PTx[j % 2], B[f"PT{j % 2}"]
                tsl = slice(tb * 512, (tb + 1) * 512)
                for mt in range(2):
                    pS, BS = bank()
                    mm(pS[:, 0:512], mkT[:, h, mt * 128:(mt + 1) * 128], xq[:, tsl], True, True, [B["mkT"], Bxq], [BS])
                    act(PT[:, mt, :], pS[:, 0:512], AF.Exp, [BS], [BPT], scale=QSCALE)

            def xa_pv(j):
                h, tb = stepsx[j]
                PT, BPT = PTx[j % 2], B[f"PT{j % 2}"]
                rcb, Brc = rcx[j % 2], B[f"rc{j % 2}"]
                tsl = slice(tb * 512, (tb + 1) * 512)
                pN, BN = bank()
                pD, BD = bank()
                for mt in range(2):
                    mm(pN[:, 0:512], mvv[:, mt, h * 128:(h + 1) * 128], PT[:, mt, :], mt == 0, mt == 1, [B["mv"], BPT], [BN])
                for mt in range(2):
                    mm(pD[:, 0:512], ones_b, PT[:, mt, :], mt == 0, mt == 1, [B_ident, BPT], [BD])
                DVE.op(lambda: nc.vector.reciprocal(out=rcb, in_=pD[:, 0:512]), [BD], [Brc])
                tt("dve", xaT[:, h, tsl], pN[:, 0:512], rcb, ALU.mult, [BN, Brc], [B_xaT])

            xa_scores(0)
            for j in range(len(stepsx)):
                if j + 1 < len(stepsx):
                    xa_scores(j + 1)
                xa_pv(j)
            barrier()
            WAR.release(mW)
            WORK.release(m0)

        xattn()
        dump("xaT", xaT, [B_xaT], [128, 4, S])
        if stop_after == "xa":
            return _finish(nc, SP, dumps)

        mW6 = WAR.mark()
        B_mT = [Buf(f"mT{i}") for i in range(4)]
        B_sg = [Buf("sg0"), Buf("sg1")]
        B_mt = [Buf("mt0"), Buf("mt1")]
        srcs = [(zT, B_zT, 8, 0), (oT, B_oT, 4, 8), (xaT, B_xaT, 4, 12)]
        it = 0
        wout = wg6[0][:].rearrange("p a b -> p (a b)")[:, 0:KC * D].rearrange("p (k f) -> p k f", k=KC)
        B_wout = B_w6[0]
        w_out_v = w_out_d.rearrange("(k p) f -> p k f", p=128)
        scale_w6(0)
        for c in range(KC):
            if c % 2 == 0 and c // 2 + 1 < KC // 2:
                load_w6(c // 2 + 1)
            if c % 2 == 1 and c // 2 + 1 < KC // 2:
                scale_w6(c // 2 + 1)
            if c == 6:
                qpl.dma(wout[:, :, 0:512], w_out_v[:, :, 0:512], writes=[B_wout])
                qpl.dma(wout[:, :, 512:1024], w_out_v[:, :, 512:1024], writes=[B_wout])
            sl, Bw = wg6[(c // 2) % 2], B_w6[(c // 2) % 2]
            co = (c % 2) * 128
            for tb in range(4):
                tsl = slice(tb * 512, (tb + 1) * 512)
                for b_, (src, Bsrc, nk, woff) in enumerate(srcs):
                    pY, BY = bank()
                    pG, BG = bank()
                    for k in range(nk):
                        mm(pY[:, 0:512], sl[:, woff + k, co:co + 128], src[:, k, tsl], k == 0, k == nk - 1, [Bw, Bsrc], [BY])
                    for k in range(KC):
                        mm(pG[:, 0:512], sl[:, 16 + 8 * b_ + k, co:co + 128], hT[:, k, tsl], k == 0, k == KC - 1, [Bw, B_hT[tb]], [BG])
                    s_, Bs_ = sgt[it % 2], B_sg[it % 2]
                    it += 1
                    act(s_, pG[:, 0:512], AF.Sigmoid, [BG], [Bs_])
                    if b_ == 0:
                        tt("dve", mtmp[0], pY[:, 0:512], s_, ALU.mult, [BY, Bs_], [B_mt[0]])
                    elif b_ == 1:
                        tt("dve", mtmp[1], pY[:, 0:512], s_, ALU.mult, [BY, Bs_], [B_mt[1]])
                        tt("pool", mtmp[0], mtmp[0], mtmp[1], ALU.add, [B_mt[0], B_mt[1]], [B_mt[0]])
                    else:
                        tt("dve", mtmp[1], pY[:, 0:512], s_, ALU.mult, [BY, Bs_], [B_mt[1]])
                        tt("pool", mT[:, c, tsl], mtmp[0], mtmp[1], ALU.add, [B_mt[0], B_mt[1]], [B_mT[tb]])
        dump("mT", mT, B_mT, [128, KC, S])
        if stop_after == "merge":
            return _finish(nc, SP, dumps)
        assert not PE.pending
        pe_done = Tok(PE.sem, PE.count)
        pool_done = Tok(POOL.sem, POOL.count)
        BIG.release(BIG.lo)
        x2 = BIG.alloc([NT, D], F32)
        B_x2 = [Buf(f"x2_{i}") for i in range(NT)]
        WORK.release(m0)
        xs7f = wg6[1][:].rearrange("p a b -> p (a b)").bitcast(F32)
        xs7 = [xs7f[:, 0:D], xs7f[:, D:2 * D]]
        B_xs7 = [Buf("xs7a"), Buf("xs7b")]
        SP.wait(pe_done)
        DVE.wait(pe_done)
        DVE.wait(pool_done)
        for i in range(NT):
            xt, Bx = xs7[i % 2], B_xs7[i % 2]
            qsp.dma(xt, x_d[i * 128:(i + 1) * 128, :], writes=[Bx])
            for half in range(2):
                pb, Bp = bank()
                for k in range(KC):
                    mm(pb[:, 0:512], mT[:, k, i * 128:(i + 1) * 128], wout[:, k, half * 512:(half + 1) * 512], k == 0, k == KC - 1,
                       [B_mT[i // 4], B_wout], [Bp])
                tt("dve", x2[:, i, half * 512:(half + 1) * 512], pb[:, 0:512], xt[:, half * 512:(half + 1) * 512], ALU.add, [Bp, Bx], [B_x2[i]])
        dump("x2", x2, B_x2, [128, NT, D])
        if stop_after == "x2":
            return _finish(nc, SP, dumps)
        barrier()
        WAR.release(mW6)

        tT = hT
        B_tT = [Buf(f"tT{i}") for i in range(4)]
        wsl8 = [WAR.alloc([12288], BF16) for _ in range(2)]
        B_w8 = [Buf("w8a"), Buf("w8b")]
        W8 = Arena(arena_t, WORK.lo, WORK.hi)
        wr = W8.alloc([KC, 20], F32)
        rb = W8.alloc([20], F32)
        lgl = W8.alloc([NT, 20], F32)
        comb = W8.alloc([NT, 16], F32)
        off_t = W8.top
        tst = [W8.alloc([KC, 128], F32) for _ in range(1)]
        W8b = Arena(arena_t, off_t, off_t + 4 * KB)
        thi = W8b.alloc([D], BF16)
        tlo = W8b.alloc([D], BF16)
        tlT = W8.alloc([KC, 128], BF16)
        whi = W8.alloc([KC, 20], BF16)
        wlo = W8.alloc([KC, 20], BF16)
        tb_ = [W8.alloc([D], F32) for _ in range(1)]
        junk8 = W8.alloc([D], BF16)
        rt = {n: W8.alloc([NT, 4], F32) for n in ["ohg", "eg", "ig", "eq", "ee", "w"]}
        rt4 = W8.alloc([NT, 4, 4], F32)
        r1 = {n: W8.alloc([NT], F32) for n in ["gmax", "gsum", "m1", "m2", "se"]}
        hid = [W8.alloc([4, 512], BF16) for _ in range(2)]
        sa2 = W8.alloc([1024], BF16)
        sa = [sa2[:, 0:512], sa2[:, 512:1024]]
        B8 = {n: Buf(n) for n in ["wr", "rb", "lgl", "comb", "tst", "tb", "junk", "rt", "hid0", "hid1", "sa0", "sa1", "thi", "tlo", "tlT"]}
        w_eg_v = w_eg_d.rearrange("e (k p) f -> e p k f", p=128)
        w_eu_v = w_eu_d.rearrange("e (k p) f -> e p k f", p=128)
        w_ed_v = w_ed_d.rearrange("e (k p) f -> e p k f", p=128)

        def load_w8(e):
            sl, Bw = wsl8[e % 2], B_w8[e % 2]
            qpl.dma(sl[:, 0:4096].rearrange("p (k f) -> p k f", k=8), w_eg_v[e], writes=[Bw])
            qpl.dma(sl[:, 4096:8192].rearrange("p (k f) -> p k f", k=8), w_eu_v[e], writes=[Bw])
            qpl.dma(sl[:, 8192:12288].rearrange("p (k f) -> p k f", k=4), w_ed_v[e], writes=[Bw])

        if not lite:
            load_w8(0)
        qsp.dma(gA, g_ffn_d.partition_broadcast(128), writes=[B_gA])
        import os
        if os.environ.get("SKIPWR") != "1":
            qsp.dma(wr[:, :, 0:4], w_rg_d.rearrange("(k p) f -> p k f", p=128), writes=[B8["wr"]])
            qsp.dma(wr[:, :, 4:20], w_re_d.rearrange("(k p) f -> p k f", p=128), writes=[B8["wr"]])
            qsp.dma(rb[:, 0:4], b_rg_d.partition_broadcast(128), writes=[B8["rb"]])
            qsp.dma(rb[:, 4:20], b_re_d.partition_broadcast(128), writes=[B8["rb"]])
        cp("dve", whi, wr, [B8["wr"]], [B8["wr"]])
        tt("dve", wlo, wr, whi, ALU.subtract, [B8["wr"]], [B8["wr"]])
        hidf = [hid[j][:].rearrange("p a b -> p (a b)") for j in range(2)]
        tbs = [tb_[0], hidf[0].bitcast(F32)]
        B_tbs = [[B8["tb"]], [B8["hid0"]]]
        this = [thi, hidf[1][:, 0:D]]
        tlos = [tlo, hidf[1][:, D:2 * D]]
        B_this = [[B8["thi"]], [B8["hid1"]]]
        B_tlos = [[B8["tlo"]], [B8["hid1"]]]
        tlTs = [tlT, sa2.rearrange("p (k t) -> p k t", k=KC)]
        B_tlTs = [[B8["tlT"]], [B8["sa0"], B8["sa1"]]]

        def route_stage1(i):
            u = i % 2
            tbu, thu, tlu = tbs[u], this[u], tlos[u]
            Btb, Bth, Btl = B_tbs[u], B_this[u], B_tlos[u]
            ss = small[:, 8 + (i % 8):9 + (i % 8)]
            Bs = B_ss[8 + i % 8]
            act(junk8, x2[:, i, :], AF.Square, [B_x2[i]], [B8["junk"], Bs], accum_out=ss)
            act(ss, ss, AF.Sqrt, [Bs], [Bs], scale=1.0 / D, bias=1e-6)
            DVE.op(lambda: nc.vector.reciprocal(out=ss, in_=ss), [Bs], [Bs])
            stt(tbu, x2[:, i, :], ss, gA, ALU.mult, ALU.mult, [B_x2[i], Bs, B_gA], Btb)
            cp("act", thu, tbu, Btb, Bth)
            tt("dve", tlu, tbu, thu, ALU.subtract, Btb + Bth, Btl)

        def route_stage1b(i):
            u = i % 2
            thu, tlu = this[u], tlos[u]
            Bth, Btl = B_this[u], B_tlos[u]
            pb, Bp = bank()
            pT = pb[:].bitcast(BF16)
            for k in range(KC):
                tr(pT[:, k * 128:(k + 1) * 128], thu[:, k * 128:(k + 1) * 128], ident_b, Bth + [B_ident], [Bp], k == KC - 1)
            cp("act", tT[:, :, i * 128:(i + 1) * 128], pT.rearrange("p (k t) -> p k t", k=KC), [Bp], [B_tT[i // 4]])
            pb, Bp = bank()
            pT = pb[:].bitcast(BF16)
            for k in range(KC):
                tr(pT[:, k * 128:(k + 1) * 128], tlu[:, k * 128:(k + 1) * 128], ident_b, Btl + [B_ident], [Bp], k == KC - 1)
            cp("dve", tlTs[u], pT.rearrange("p (k t) -> p k t", k=KC), [Bp], B_tlTs[u])

        def route_stage2(i):
            u = i % 2
            pb, Bp = bank()
            nmm = 0
            for (lt, wv_) in [("hi", whi), ("lo", whi), ("hi", wlo)]:
                for k in range(KC):
                    lhs = tT[:, k, i * 128:(i + 1) * 128] if lt == "hi" else tlTs[u][:, k, :]
                    Rl = ([B_tT[i // 4]] if lt == "hi" else B_tlTs[u]) + [B8["wr"]]
                    mm(pb[:, 0:20], lhs, wv_[:, k, :], nmm == 0, nmm == 3 * KC - 1, Rl, [Bp])
                    nmm += 1
            tt("dve", lgl[:, i, :], pb[:, 0:20], rb, ALU.add, [Bp, B8["rb"]], [B8["lgl"]])

        route_stage1(0)
        for i in range(NT):
            if i + 1 < NT:
                route_stage1(i + 1)
            route_stage1b(i)
            if i >= 1:
                route_stage2(i - 1)
        route_stage2(NT - 1)
        if stop_after == "moe_lg":
            dump("lgl", lgl, [B8["lgl"]], [128, NT, 20])
            return _finish(nc, SP, dumps)
        Lg = lgl[:, :, 0:4]
        Le = lgl[:, :, 4:20].rearrange("p t (g e) -> p t g e", g=4)
        RB = [B8["lgl"], B8["rt"]]
        WB_ = [B8["rt"]]

        def bc4(ap):
            return ap.unsqueeze(2).broadcast_to([128, NT, 4])

        red(r1["gmax"], Lg, ALU.max, RB, WB_)
        tt("dve", rt["ohg"], Lg, bc4(r1["gmax"]), ALU.is_equal, RB, WB_)
        tt("dve", rt["eg"], Lg, bc4(r1["gmax"]), ALU.subtract, RB, WB_)
        act(rt["eg"], rt["eg"], AF.Exp, RB, WB_)
        red(r1["gsum"], rt["eg"], ALU.add, RB, WB_)
        tt("dve", rt4, Le, rt["ohg"].unsqueeze(3).broadcast_to([128, NT, 4, 4]), ALU.mult, RB, WB_)
        red(rt["ig"], rt4.rearrange("p t g e -> p t e g"), ALU.add, RB, WB_)
        red(r1["m1"], rt["ig"], ALU.max, RB, WB_)
        tt("dve", rt["eq"], rt["ig"], bc4(r1["m1"]), ALU.is_equal, RB, WB_)
        stt(rt["eq"], rt["eq"], -1e30, rt["ig"], ALU.mult, ALU.add, RB, WB_)
        red(r1["m2"], rt["eq"], ALU.max, RB, WB_)
        tt("dve", rt["eq"], rt["ig"], bc4(r1["m2"]), ALU.is_ge, RB, WB_)
        tt("dve", rt["ee"], rt["ig"], bc4(r1["m1"]), ALU.subtract, RB, WB_)
        act(rt["ee"], rt["ee"], AF.Exp, RB, WB_)
        tt("dve", rt["ee"], rt["ee"], rt["eq"], ALU.mult, RB, WB_)
        red(r1["se"], rt["ee"], ALU.add, RB, WB_)
        tt("dve", r1["se"], r1["se"], r1["gsum"], ALU.mult, RB, WB_)
        DVE.op(lambda: nc.vector.reciprocal(out=r1["se"], in_=r1["se"]), RB, WB_)
        tt("dve", rt["w"], rt["ee"], bc4(r1["se"]), ALU.mult, RB, WB_)
        tt("dve", comb[:].rearrange("p t (g e) -> p t g e", g=4), rt["ohg"].unsqueeze(3).broadcast_to([128, NT, 4, 4]),
           rt["w"].unsqueeze(2).broadcast_to([128, NT, 4, 4]), ALU.mult, RB, [B8["comb"]])
        dump("comb", comb, [B8["comb"]], [128, NT, 16])
        dump("tT", tT, B_tT, [128, KC, S])
        if stop_after == "route":
            return _finish(nc, SP, dumps)
        assert not PE.pending
        route_pe_tok = Tok(PE.sem, PE.count)
        qsp.dma(gB, g_fin_d.partition_broadcast(128), writes=[B_gB])
        ob = [tst[0].rearrange("p k t -> p (k t)"), tb_[0]]
        B_ob = [B8["tst"], B8["tb"]]
        outs = []

        def final_tile(i):
            ss = small[:, 16 + (i % 8):17 + (i % 8)]
            act(junk8, x2[:, i, :], AF.Square, [B_x2[i]], [B8["junk"], B_small], accum_out=ss)
            act(ss, ss, AF.Sqrt, [B_small], [B_small], scale=1.0 / D, bias=1e-6)
            DVE.op(lambda: nc.vector.reciprocal(out=ss, in_=ss), [B_small], [B_small])
            stt(ob[i % 2], x2[:, i, :], ss, gB, ALU.mult, ALU.mult, [B_x2[i], B_small, B_gB], [B_ob[i % 2]])
            outs.append(qsp.dma(out_d[i * 128:(i + 1) * 128, :], ob[i % 2], reads=[B_ob[i % 2]]))

        itc = [0]

        def wviews(e):
            sl, Bw = wsl8[e % 2], B_w8[e % 2]
            wgv = sl[:, 0:4096].rearrange("p (k f) -> p k f", k=8)
            wuv = sl[:, 4096:8192].rearrange("p (k f) -> p k f", k=8)
            wdv = sl[:, 8192:12288].rearrange("p (k f) -> p k f", k=4)
            return wgv, wuv, wdv, Bw

        def gate_up(e, tb):
            wgv, wuv, wdv, Bw = wviews(e)
            tsl = slice(tb * 512, (tb + 1) * 512)
            hd, Bhd = hid[tb % 2], B8[f"hid{tb % 2}"]
            for fc in range(4):
                pA, BA = bank()
                pU, BU = bank()
                for k in range(KC):
                    mm(pA[:, 0:512], wgv[:, k, fc * 128:(fc + 1) * 128], tT[:, k, tsl], k == 0, k == KC - 1, [Bw, B_tT[tb]], [BA])
                for k in range(KC):
                    mm(pU[:, 0:512], wuv[:, k, fc * 128:(fc + 1) * 128], tT[:, k, tsl], k == 0, k == KC - 1, [Bw, B_tT[tb]], [BU])
                s_, Bs_ = sa[itc[0] % 2], B8[f"sa{itc[0] % 2}"]
                itc[0] += 1
                act(s_, pA[:, 0:512], AF.Silu, [BA], [Bs_])
                tt("dve", hd[:, fc, :], pU[:, 0:512], s_, ALU.mult, [BU, Bs_], [Bhd])

        def down(e, tb):
            wgv, wuv, wdv, Bw = wviews(e)
            hd, Bhd = hid[tb % 2], B8[f"hid{tb % 2}"]
            for tl in range(4):
                i = tb * 4 + tl
                for half in range(2):
                    pb, Bp = bank()
                    for fc in range(4):
                        mm(pb[:, 0:512], hd[:, fc, tl * 128:(tl + 1) * 128], wdv[:, fc, half * 512:(half + 1) * 512], fc == 0, fc == 3,
                           [Bhd, Bw], [Bp])
                    xs_ = x2[:, i, half * 512:(half + 1) * 512]
                    stt(xs_, pb[:, 0:512], comb[:, i, e:e + 1], xs_, ALU.mult, ALU.add, [Bp, B8["comb"], B_x2[i]], [B_x2[i]])
            if e == 15:
                if tb == 0:
                    DVE.wait(route_pe_tok)
                for tl in range(4):
                    final_tile(tb * 4 + tl)

        steps = [(e, tb) for e in range(16) for tb in range(4)]
        load_w8(1)
        gate_up(*steps[0])
        for si, (e, tb) in enumerate(steps):
            if si + 1 < len(steps):
                gate_up(*steps[si + 1])
            down(e, tb)
            if tb == 3 and e + 2 < 16:
                load_w8(e + 2)
        dump("x3", x2, B_x2, [128, NT, D])
        for t_ in outs + dumps:
            SP.wait(t_)
        stats_ = {e.name: (e.nins, e.count) for e in [PE, ACT, DVE, POOL, SP]}
        build.stats = stats_
    return nc


def _finish(nc, SP, dumps):
    for t_ in dumps:
        SP.wait(t_)
    return nc


_CONSTS = None


def make_in_maps(inputs, lite=False):
    global _CONSTS
    if _CONSTS is None:
        _CONSTS = _host_constants()
    f = lambda a: np.ascontiguousarray(np.asarray(a, dtype=np.float32))
    shared = {
        "g_mix": f(inputs["g_mix"]).reshape(D),
        "w_in": f(inputs["w_in"]).reshape(D, 8192),
        "ret_decay_fwd": f(inputs["ret_decay_fwd"]).reshape(4),
        "ret_decay_bwd": f(inputs["ret_decay_bwd"]).reshape(4),
        "ret_norm_gain": f(inputs["ret_norm_gain"]).reshape(D),
        "w_ret_o": f(inputs["w_ret_o"]).reshape(D, D),
        "rpbT": _rpb_layout(f(inputs["na_rpb"]).reshape(8, 15, 31)),
        "w_na_o": f(inputs["w_na_o"]).reshape(512, D),
        "g_mem": f(inputs["g_mem"]).reshape(D),
        "w_mem_kv": f(inputs["w_mem_kv"]).reshape(D, D),
        "w_xa_o": f(inputs["w_xa_o"]).reshape(512, D),
        "w_out": f(inputs["w_out"]).reshape(D, D),
        "g_ffn": f(inputs["g_ffn"]).reshape(D),
        "w_router_group": f(inputs["w_router_group"]).reshape(D, 4),
        "b_router_group": f(inputs["b_router_group"]).reshape(4),
        "w_router_expert": f(inputs["w_router_expert"]).reshape(D, 16),
        "b_router_expert": f(inputs["b_router_expert"]).reshape(16),
        "w_exp_gate": f(inputs["w_exp_gate"]).reshape(16, D, 512),
        "w_exp_up": f(inputs["w_exp_up"]).reshape(16, D, 512),
        "w_exp_down": f(inputs["w_exp_down"]).reshape(16, 512, D),
        "g_final": f(inputs["g_final"]).reshape(D),
    }
    if lite:
        shared["w_exp_gate"] = np.zeros((1, 128, 512), np.float32)
        shared["w_exp_up"] = np.zeros((1, 128, 512), np.float32)
        shared["w_exp_down"] = np.zeros((1, 128, D), np.float32)
    w_in = shared["w_in"]
    wi = w_in.reshape(KC, 128, 8192)
    def blk(w, lo, hi):
        return w[:, :, lo:hi].transpose(1, 0, 2)
    w6 = np.empty((4, 128, 40, 256), np.float32)
    wro = shared["w_ret_o"].reshape(8, 128, D)
    wna = shared["w_na_o"].reshape(4, 128, D)
    wxa = shared["w_xa_o"].reshape(4, 128, D)
    for cp_ in range(4):
        lo, hi = cp_ * 256, (cp_ + 1) * 256
        w6[cp_, :, 0:8] = blk(wro, lo, hi)
        w6[cp_, :, 8:12] = blk(wna, lo, hi)
        w6[cp_, :, 12:16] = blk(wxa, lo, hi)
        for b_ in range(3):
            w6[cp_, :, 16 + 8 * b_:24 + 8 * b_] = blk(wi, 5120 + b_ * 1024 + lo, 5120 + b_ * 1024 + hi)
    shared["w6"] = w6.reshape(4, 128, 40 * 256)
    wret = np.empty((4, 128, KC, 768), np.float32)
    for h in range(4):
        wret[h, :, :, 0:128] = blk(wi, h * 128, (h + 1) * 128)
        wret[h, :, :, 128:256] = blk(wi, 512 + h * 128, 512 + (h + 1) * 128)
        wret[h, :, :, 256:512] = blk(wi, 1024 + h * 256, 1024 + (h + 1) * 256)
        wret[h, :, :, 512:768] = blk(wi, 2048 + h * 256, 2048 + (h + 1) * 256)
    shared["wret"] = wret.reshape(4, 128, KC * 768)
    shared["gn_pk"] = np.ascontiguousarray(shared["ret_norm_gain"].reshape(KC, 128).T)
    shared.update(_CONSTS)
    x = f(inputs["x"])
    mem = f(inputs["mem"])
    maps = []
    for b in range(x.shape[0]):
        m = dict(shared)
        m["x"] = x[b]
        m["mem"] = mem[b]
        maps.append(m)
    return maps


def kernel(**inputs):
    nc = build()
    in_maps = make_in_maps(inputs)
    res = run_bass_kernel_spmd(nc, in_maps, core_ids=list(range(len(in_maps))))
    out = np.stack([np.asarray(r["out"], dtype=np.float32) for r in res.results], axis=0)
    return out
```
